# Optimizing a Trainium2 kernel written in Bass

```python
import math
import jax, jax.numpy as jnp
from jax import lax
import numpy as np

D_MODEL = 1024
BATCH = 2
SEQ = 16384
DEPTH = 2

N_META = 16
D_RNN = D_MODEL
RG_BLOCKS = 4
RG_BLOCK_W = D_RNN // RG_BLOCKS
CONV_W = 4
LRU_C = 8.0
N_HEADS = 8
HEAD_DIM = D_MODEL // N_HEADS
D_ATTN = N_HEADS * HEAD_DIM
IDX_HEADS = 8
IDX_DIM = 64
TOP_K_MAX = 256
Q_BLOCK = 128
N_BUCKETS = 32
MAX_DISTANCE = 128
D_FF = 4 * D_MODEL
NORM_EPS = 1e-6

SZ_RG_GATE = D_RNN
SZ_RG_X = D_RNN
SZ_Q = D_ATTN
SZ_K = D_ATTN
SZ_V = D_ATTN
SZ_QI = IDX_HEADS * IDX_DIM
SZ_KI = IDX_DIM
SZ_WI = IDX_HEADS
SZ_G_RNN = D_MODEL
SZ_G_ATTN = D_MODEL
IN_SIZES = (SZ_RG_GATE, SZ_RG_X, SZ_Q, SZ_K, SZ_V, SZ_QI, SZ_KI, SZ_WI, SZ_G_RNN, SZ_G_ATTN)
N_IN = sum(IN_SIZES)
SPLIT_POINTS = tuple(int(s) for s in np.cumsum(IN_SIZES)[:-1])

kernel_name = "hybrid_rglru_dsa_gated_block"


def rms_norm(x, g):
    xf = x.astype(jnp.float32)
    y = xf * lax.rsqrt(jnp.mean(xf * xf, axis=-1, keepdims=True) + NORM_EPS)
    return (y * g.astype(jnp.float32)).astype(x.dtype)


def causal_depthwise_conv(x, w, b):
    L = x.shape[1]
    xp = jnp.pad(x, ((0, 0), (CONV_W - 1, 0), (0, 0)))
    y = w[0] * xp[:, 0:L]
    for j in range(1, CONV_W):
        y = y + w[j] * xp[:, j:j + L]
    return y + b


def block_diag_linear(x, w, b):
    B, L, _ = x.shape
    xg = x.reshape(B, L, RG_BLOCKS, RG_BLOCK_W)
    return jnp.einsum('blgi,gij->blgj', xg, w).reshape(B, L, D_RNN) + b


def rg_lru(x, w_a, b_a, w_x, b_x, lam):
    r = jax.nn.sigmoid(block_diag_linear(x, w_a, b_a)).astype(jnp.float32)
    i = jax.nn.sigmoid(block_diag_linear(x, w_x, b_x)).astype(jnp.float32)
    log_a = -LRU_C * r * jax.nn.softplus(-lam.astype(jnp.float32))
    a = jnp.exp(log_a)
    u = jnp.sqrt(-jnp.expm1(2.0 * log_a)) * (i * x.astype(jnp.float32))

    def combine(c1, c2):
        a1, b1 = c1
        a2, b2 = c2
        return a1 * a2, a2 * b1 + b2

    _, h = lax.associative_scan(combine, (a, u), axis=1)
    return h.astype(x.dtype)


def t5_causal_bucket(dist):
    max_exact = N_BUCKETS // 2
    d = jnp.maximum(dist, 0)
    df = jnp.maximum(d, 1).astype(jnp.float32)
    large = max_exact + (jnp.log(df / max_exact) / math.log(MAX_DISTANCE / max_exact)
                         * (N_BUCKETS - max_exact)).astype(jnp.int32)
    large = jnp.minimum(large, N_BUCKETS - 1)
    return jnp.where(d < max_exact, d, large)


def sparse_indexed_attention(q, k, v, q_idx, k_idx, w_idx, rel_bias):
    B, L = q.shape[0], q.shape[1]
    top_k = min(TOP_K_MAX, L // 4)
    n_blocks = -(-L // Q_BLOCK)
    Lp = n_blocks * Q_BLOCK

    def pad(a):
        return jnp.pad(a, ((0, 0), (0, Lp - L)) + ((0, 0),) * (a.ndim - 2))

    qp, qip, wp = pad(q), pad(q_idx), pad(w_idx)
    key_pos = jnp.arange(L, dtype=jnp.int32)
    scale = HEAD_DIM ** -0.5
    gather = jax.vmap(lambda a, idx: a[idx])

    def one_block(blk):
        start = blk * Q_BLOCK
        qb = lax.dynamic_slice_in_dim(qp, start, Q_BLOCK, axis=1)
        qib = lax.dynamic_slice_in_dim(qip, start, Q_BLOCK, axis=1)
        wb = lax.dynamic_slice_in_dim(wp, start, Q_BLOCK, axis=1)
        t = start + jnp.arange(Q_BLOCK, dtype=jnp.int32)
        s_raw = jnp.einsum('bqhd,bkd->bqhk', qib, k_idx)
        score = jnp.einsum('bqhk,bqh->bqk', jax.nn.relu(s_raw), wb).astype(jnp.float32)
        causal = key_pos[None, None, :] <= t[None, :, None]
        score = jnp.where(causal, score, -jnp.inf)
        _, sel = lax.top_k(score, top_k)
        k_sel = gather(k, sel)
        v_sel = gather(v, sel)
        bias = rel_bias[t5_causal_bucket(t[None, :, None] - sel)]
        logits = (jnp.einsum('bqhd,bqkhd->bqhk', qb, k_sel).astype(jnp.float32) * scale
                  + jnp.moveaxis(bias, -1, 2).astype(jnp.float32))
        valid = (sel <= t[None, :, None])[:, :, None, :]
        logits = jnp.where(valid, logits, -jnp.inf)
        p = jax.nn.softmax(logits, axis=-1).astype(v.dtype)
        return jnp.einsum('bqhk,bqkhd->bqhd', p, v_sel)

    out = lax.map(one_block, jnp.arange(n_blocks, dtype=jnp.int32))
    out = jnp.moveaxis(out, 0, 1).reshape(B, Lp, N_HEADS, HEAD_DIM)[:, :L]
    return out


def hybrid_layer(h, norm1_g, w_in, conv_w, conv_b, w_rg_a, b_rg_a, w_rg_x, b_rg_x,
                 lru_lambda, w_out, norm2_g, w_mlp1, w_mlp2, rel_bias):
    B, L, _ = h.shape
    xn = rms_norm(h, norm1_g)
    proj = xn @ w_in
    (u_gate, u_rnn, q, k, v, qi, ki, wi, g_rnn, g_attn) = jnp.split(proj, SPLIT_POINTS, axis=-1)
    xc = causal_depthwise_conv(u_rnn, conv_w, conv_b)
    y_rnn = rg_lru(xc, w_rg_a, b_rg_a, w_rg_x, b_rg_x, lru_lambda) * jax.nn.gelu(u_gate)
    y_attn = sparse_indexed_attention(
        q.reshape(B, L, N_HEADS, HEAD_DIM), k.reshape(B, L, N_HEADS, HEAD_DIM),
        v.reshape(B, L, N_HEADS, HEAD_DIM), qi.reshape(B, L, IDX_HEADS, IDX_DIM),
        ki, wi, rel_bias).reshape(B, L, D_ATTN)
    y = jax.nn.sigmoid(g_rnn) * y_rnn + jax.nn.sigmoid(g_attn) * y_attn
    h = h + y @ w_out
    hn = rms_norm(h, norm2_g)
    h = h + jnp.square(jax.nn.relu(hn @ w_mlp1)) @ w_mlp2
    return h


def setup_inputs(seed: int = 0) -> dict:
    key = jax.random.key(seed)
    ks = jax.random.split(key, 20)
    f32 = jnp.float32
    x = jax.random.normal(ks[0], (BATCH, SEQ, D_MODEL), f32)
    norm1_g = 1.0 + 0.01 * jax.random.normal(ks[1], (DEPTH, D_MODEL), f32)
    w_in = jax.random.normal(ks[2], (DEPTH, D_MODEL, N_IN), f32) * D_MODEL ** -0.5
    conv_w = jax.random.normal(ks[3], (DEPTH, CONV_W, D_RNN), f32) * CONV_W ** -0.5
    conv_b = 0.01 * jax.random.normal(ks[4], (DEPTH, D_RNN), f32)
    w_rg_a = jax.random.normal(ks[5], (DEPTH, RG_BLOCKS, RG_BLOCK_W, RG_BLOCK_W), f32) * RG_BLOCK_W ** -0.5
    b_rg_a = 0.01 * jax.random.normal(ks[6], (DEPTH, D_RNN), f32)
    w_rg_x = jax.random.normal(ks[7], (DEPTH, RG_BLOCKS, RG_BLOCK_W, RG_BLOCK_W), f32) * RG_BLOCK_W ** -0.5
    b_rg_x = 0.01 * jax.random.normal(ks[8], (DEPTH, D_RNN), f32)
    a0 = jax.random.uniform(ks[9], (DEPTH, D_RNN), f32, minval=0.9, maxval=0.999)
    lru_lambda = jnp.log(a0) - jnp.log1p(-a0)
    w_out = jax.random.normal(ks[10], (DEPTH, D_MODEL, D_MODEL), f32) * D_MODEL ** -0.5
    norm2_g = 1.0 + 0.01 * jax.random.normal(ks[11], (DEPTH, D_MODEL), f32)
    w_mlp1 = jax.random.normal(ks[12], (DEPTH, D_MODEL, D_FF), f32) * D_MODEL ** -0.5
    w_mlp2 = jax.random.normal(ks[13], (DEPTH, D_FF, D_MODEL), f32) * D_FF ** -0.5
    rel_bias = 0.1 * jax.random.normal(ks[14], (N_BUCKETS, N_HEADS), f32)
    meta_tokens = jax.random.normal(ks[15], (N_META, D_MODEL), f32)
    final_g = 1.0 + 0.01 * jax.random.normal(ks[16], (D_MODEL,), f32)
    return {"x": x, "norm1_g": norm1_g, "w_in": w_in, "conv_w": conv_w, "conv_b": conv_b,
            "w_rg_a": w_rg_a, "b_rg_a": b_rg_a, "w_rg_x": w_rg_x, "b_rg_x": b_rg_x,
            "lru_lambda": lru_lambda, "w_out": w_out, "norm2_g": norm2_g,
            "w_mlp1": w_mlp1, "w_mlp2": w_mlp2, "rel_bias": rel_bias,
            "meta_tokens": meta_tokens, "final_g": final_g}


def reference(x, norm1_g, w_in, conv_w, conv_b, w_rg_a, b_rg_a, w_rg_x, b_rg_x,
              lru_lambda, w_out, norm2_g, w_mlp1, w_mlp2, rel_bias, meta_tokens, final_g):
    B = x.shape[0]
    meta = jnp.broadcast_to(meta_tokens[None].astype(x.dtype), (B, N_META, D_MODEL))
    h = jnp.concatenate([meta, x], axis=1)
    for l in range(DEPTH):
        h = hybrid_layer(h, norm1_g[l], w_in[l], conv_w[l], conv_b[l], w_rg_a[l], b_rg_a[l],
                         w_rg_x[l], b_rg_x[l], lru_lambda[l], w_out[l], norm2_g[l],
                         w_mlp1[l], w_mlp2[l], rel_bias)
    h = rms_norm(h, final_g)
    return h[:, N_META:]
```

```python
import numpy as np
from contextlib import ExitStack
import ml_dtypes
import concourse.bass as bass
import concourse.mybir as mybir
from concourse.bass_utils import run_bass_kernel_spmd

F32 = mybir.dt.float32
BF16 = mybir.dt.bfloat16
AF = mybir.ActivationFunctionType
ALU = mybir.AluOpType
AX = mybir.AxisListType

D = 1024
NB = 2
SEQ = 16384
NMETA = 16
L = SEQ + NMETA
NSLOT = 33
NTILE = 4 * NSLOT
LP = NTILE * 128
TOK = NSLOT * 128
NIN = 7752
DFF = 4096
TOPK = 256
EPS = 1e-6
NCORE = 8


class Buf:
    __slots__ = ("lw", "rd")

    def __init__(self):
        self.lw = None
        self.rd = {}


_ALL_CHANS = []


class Chan:
    __slots__ = ("sem", "count", "retired", "cc")

    def __init__(self):
        self.sem = None
        self.count = 0
        self.retired = []
        self.cc = False
        _ALL_CHANS.append(self)


class Sched:
    LIMIT = 30000
    ENG = ("pe", "act", "dve", "pool", "sp")

    def __init__(self, nc, es):
        self.nc = nc
        self.es = es
        self.streams = {e: [] for e in self.ENG}
        self.cur = {}
        self.waited = {e: {} for e in self.ENG}
        self.nsem = 0
        self.sempool = []
        self.ccpool = []

    def newsem(self):
        self.nsem += 1
        return self.es.enter_context(self.nc.semaphore(f"sm{self.nsem}"))

    def _chan_acquire(self, chan, inc):
        if chan.sem is None or chan.count + inc > self.LIMIT:
            if chan.sem is not None:
                chan.retired.append((chan.sem, chan.count))
            pool = self.ccpool if chan.cc else self.sempool
            for k, (sm, cn) in enumerate(pool):
                if cn + 64 * inc <= self.LIMIT:
                    del pool[k]
                    chan.sem, chan.count = sm, cn
                    break
            else:
                chan.sem = self.newsem()
                chan.count = 0
        chan.count += inc

    def release_chans(self):
        for ch in _ALL_CHANS:
            if ch.sem is not None:
                (self.ccpool if ch.cc else self.sempool).append((ch.sem, ch.count))
            ch.sem = None
            ch.retired = []
        del _ALL_CHANS[:]

    def _waits(self, eng, reads, writes):
        deps = {}

        def add(tok):
            if tok is None:
                return
            s, v, own = tok
            if own == "pe" and eng == "pe":
                return
            k = id(s)
            if k not in deps or deps[k][1] < v:
                deps[k] = (s, v)

        for b in reads:
            add(b.lw)
        for b in writes:
            add(b.lw)
            for t in b.rd.values():
                add(t)
        out = []
        w = self.waited[eng]
        for k, (s, v) in deps.items():
            if w.get(k, 0) >= v:
                continue
            w[k] = v
            out.append((s, v))
        return out

    @staticmethod
    def _commit(tok, reads, writes):
        k = id(tok[0])
        for b in reads:
            b.rd[k] = tok
        for b in writes:
            b.lw = tok
            b.rd = {}

    def op(self, eng, fn, reads=(), writes=()):
        waits = self._waits(eng, reads, writes)
        c = self.cur.get(eng)
        if c is None or c[1] >= self.LIMIT:
            c = [self.newsem(), 0]
            self.cur[eng] = c
        c[1] += 1
        tok = (c[0], c[1], eng)
        self.streams[eng].append((waits, fn, c[0], 1))
        self._commit(tok, reads, writes)
        return tok

    def dma(self, q, chan, out, in_, reads=(), writes=(), **kw):
        waits = self._waits(q, reads, writes)
        self._chan_acquire(chan, 16)
        tok = (chan.sem, chan.count, None)
        self.streams[q].append(
            (waits, lambda e: e.dma_start(out=out, in_=in_, **kw), chan.sem, 16))
        self._commit(tok, reads, writes)
        return tok

    def coll(self, kind, chan, ins, outs, groups, inc=1, reads=(), writes=()):
        waits = self._waits("pool", reads, writes)
        chan.cc = True
        self._chan_acquire(chan, inc)
        tok = (chan.sem, chan.count, None)
        self.streams["pool"].append(
            (waits, lambda e: e.collective_compute(kind, ALU.bypass, replica_groups=groups,
                                                   ins=ins, outs=outs), chan.sem, inc))
        self._commit(tok, reads, writes)
        return tok

    def wait_all(self, eng, bufs):
        waits = self._waits(eng, bufs, ())
        self.streams[eng].append((waits, None, None, 0))

    def barrier(self):
        toks = []
        for e in self.ENG:
            c = self.cur.get(e)
            if c is not None and c[1] > 0:
                toks.append((c[0], c[1]))
        for ch in _ALL_CHANS:
            toks.extend(ch.retired)
            if ch.sem is not None:
                toks.append((ch.sem, ch.count))
        for e in self.ENG:
            w = self.waited[e]
            waits = []
            for s_, v in toks:
                k = id(s_)
                if w.get(k, 0) >= v:
                    continue
                w[k] = v
                waits.append((s_, v))
            self.streams[e].append((waits, None, None, 0))

    def wait_chans(self, eng, chans):
        waits = []
        for ch in chans:
            waits.extend(ch.retired)
            if ch.sem is not None:
                waits.append((ch.sem, ch.count))
        self.streams[eng].append((waits, None, None, 0))

    def emit(self):
        nc = self.nc

        def run(e, name):
            for waits, fn, sem, inc in self.streams[name]:
                for s, v in waits:
                    e.wait_ge(s, v)
                if fn is not None:
                    fn(e).then_inc(sem, inc)

        with nc.Block() as block:
            block.tensor(lambda e: run(e, "pe"))
            block.scalar(lambda e: run(e, "act"))
            block.vector(lambda e: run(e, "dve"))
            block.gpsimd(lambda e: run(e, "pool"))
            block.sync(lambda e: run(e, "sp"))


class Ctx:
    def __init__(self):
        del _ALL_CHANS[:]
        self.nc = bass.Bass("TRN2", target_bir_lowering=False)
        self.es = ExitStack()
        self.pes = ExitStack()
        self.s = Sched(self.nc, self.es)
        self.n = 0

    def sb(self, shape, dt):
        self.n += 1
        return self.pes.enter_context(self.nc.sbuf_tensor(f"t{self.n}", list(shape), dt))

    def ps(self, shape, dt=F32):
        self.n += 1
        return self.pes.enter_context(self.nc.psum_tensor(f"p{self.n}", list(shape), dt))

    def end_phase(self):
        self.s.barrier()
        self.s.release_chans()
        self.pes.close()
        self.pes = ExitStack()

    def io(self, T, name, shape, dt, kind):
        if T is not None:
            return T[name]
        return self.dram(name, shape, dt, kind)

    def dram(self, name, shape, dt, kind):
        return self.nc.dram_tensor(name, list(shape), dt, kind=kind).ap()

    def finish(self):
        self.s.emit()
        self.pes.close()
        self.es.close()
        return self.nc


def run_spmd(nc, in_maps):
    res = run_bass_kernel_spmd(nc, in_maps, core_ids=list(range(NCORE)))
    return res.results


def const_identities(c):
    s = c.s
    idb = c.sb([128, 128], BF16)
    idf = c.sb([128, 128], F32)
    bb, bf = Buf(), Buf()
    for t, b in ((idb, bb), (idf, bf)):
        s.op("pool", lambda e, t=t: e.memset(t[:], 1.0), writes=[b])
        s.op("pool", lambda e, t=t: e.affine_select(
            out=t[:], in_=t[:], pattern=[[-1, 128]], compare_op=ALU.is_equal,
            fill=0.0, base=0, channel_multiplier=1), reads=[b], writes=[b])
    return idb, bb, idf, bf


NBIS = 20
SCALE = 128.0 ** -0.5


def build_C(nslot=NSLOT, nbis=NBIS, c=None, T=None):
    standalone = c is None
    if standalone:
        c = Ctx()
    s = c.s
    QT = c.io(T, "QT", [128, 8, TOK], BF16, "ExternalInput")
    QIT = c.io(T, "QIT", [128, 4, TOK], BF16, "ExternalInput")
    WI = c.io(T, "WI", [TOK, 8], F32, "ExternalInput")
    KTall = c.io(T, "KTall", [4, 128, 8, TOK], BF16, "ExternalInput")
    Vall = c.io(T, "Vall", [4, TOK, 1032], BF16, "ExternalInput")
    KITall = c.io(T, "KITall", [4, 128, TOK], BF16, "ExternalInput")
    CB = c.io(T, "CB", [128, 512], F32, "ExternalInput")
    TBG = c.io(T, "TBG", [128, 5, 8, 128], F32, "ExternalInput")
    CH = c.io(T, "CH", [128, 8], F32, "ExternalInput")
    YAT = c.io(T, "YAT", [128, 8, TOK], F32, "ExternalOutput")

    idb, b_idb, idf, b_idf = const_identities(c)
    b_ident = [b_idb, b_idf]

    NKMAX = 512 * nslot
    I_sb = c.sb([128, NKMAX], F32); b_I = Buf()
    junk = c.sb([128, NKMAX], BF16); b_junk = Buf()
    cb_sb = c.sb([128, 512], F32); b_cb = Buf()
    tbg = c.sb([128, 8, 128], F32); b_tbg = Buf()
    ch_sb = c.sb([128, 8], F32); b_ch = Buf()
    tb_sb = c.sb([128, 5, 8, 128], BF16); b_tb = Buf()
    pw2 = c.sb([128, nbis], F32); b_pw2 = Buf()
    q_sb = c.sb([128, 8, 128], BF16); b_q = Buf()
    qi_sb = c.sb([128, 4, 128], BF16); b_qi = Buf()
    wi_sb = c.sb([128, 8], F32); b_wi = Buf()
    diagw = c.sb([128, 8, 128], BF16); b_dg = Buf()
    kit = [c.sb([128, 512], BF16) for _ in range(2)]; b_kit = [Buf(), Buf()]
    rr = [c.sb([128, 512], BF16) for _ in range(8)]; b_rr = [Buf() for _ in range(8)]
    kt = [c.sb([128, 8, 512], BF16) for _ in range(2)]; b_kt = [Buf(), Buf()]
    vv = [c.sb([128, 4, 1032], BF16) for _ in range(2)]; b_vv = [Buf(), Buf()]
    mk = c.sb([128, 512], BF16); b_mk = Buf()
    mT = c.sb([128, 4, 128], BF16); b_mT = Buf()
    E = [c.sb([128, 8, 128], BF16) for _ in range(2)]
    b_E = [[Buf(), Buf()] for _ in range(2)]
    P = [c.sb([128, 8, 128], BF16) for _ in range(2)]; b_P = [Buf(), Buf()]
    y_sb = c.sb([128, 8, 128], F32); b_y = Buf()
    yT = c.sb([128, 8, 128], F32); b_yT = Buf()
    Mx = c.sb([128, 1], F32); b_Mx = Buf()
    Mi = c.sb([128, nbis], F32); b_Mi = Buf()
    cand = c.sb([128, 1], F32); b_cand = Buf()
    cnt = c.sb([128, 1], F32); b_cnt = Buf()
    dd = c.sb([128, 1], F32); b_dd = Buf()
    thr = c.sb([128, 1], F32); b_thr = Buf()
    rec = c.sb([128, 8], F32); b_rec = Buf()

    psA = [c.ps([128, 512], F32) for _ in range(3)]; b_psA = [Buf() for _ in range(3)]
    psMT = c.ps([128, 1024], BF16); b_psMT = Buf()
    psI = c.ps([128, 512], F32); b_psI = Buf()
    psO = [c.ps([128, 512], F32) for _ in range(3)]; b_psO = [Buf() for _ in range(3)]

    ch_misc = Chan()
    ch_kit = [Chan(), Chan()]
    ch_kt = [Chan(), Chan()]
    ch_vv = [Chan(), Chan()]
    ch_q = Chan()
    ch_out = Chan(); b_out = Buf()

    s.dma("sp", ch_misc, cb_sb[:], CB[:, :], writes=[b_cb])
    s.dma("sp", ch_misc, ch_sb[:], CH[:, :], writes=[b_ch])
    for j in range(nbis):
        s.op("pool", lambda e, j=j: e.memset(pw2[:, j:j + 1], 2.0 ** (-j)), writes=[b_pw2])
    for rel in range(5):
        s.dma("sp", ch_misc, tbg[:], TBG[:, rel, :, :], writes=[b_tbg])
        s.op("dve", lambda e: e.tensor_tensor(
            out=tbg[:], in0=tbg[:], in1=ch_sb[:].unsqueeze(2).to_broadcast([128, 8, 128]),
            op=ALU.subtract), reads=[b_tbg, b_ch], writes=[b_tbg])
        s.op("dve", lambda e, rel=rel: e.tensor_scalar(
            out=tb_sb[:, rel, :, :], in0=tbg[:], scalar1=float(128.0 ** 0.5), scalar2=None,
            op0=ALU.mult), reads=[b_tbg], writes=[b_tb])

    hb = [(0, 3), (3, 6), (6, 8)]
    ev = 0
    dcount = 0
    for i in range(nslot):
        nkc = i + 1
        nk = 512 * nkc
        tsl = slice(i * 128, (i + 1) * 128)
        s.dma("sp", ch_q, q_sb[:], QT[:, :, tsl], writes=[b_q])
        s.dma("sp", ch_q, qi_sb[:], QIT[:, :, tsl], writes=[b_qi])
        s.dma("sp", ch_q, wi_sb[:], WI[tsl, :], writes=[b_wi])
        for h in range(8):
            s.op("dve", lambda e, h=h: e.tensor_scalar(
                out=diagw[:, h, :], in0=idb[:], scalar1=wi_sb[:, h:h + 1], scalar2=None,
                op0=ALU.mult), reads=[b_wi, b_idb], writes=[b_dg])
        for kc in range(nkc):
            ksl = slice(kc * 128, (kc + 1) * 128)
            kb = dcount % 2
            dcount += 1
            s.dma("sp", ch_kit[kb], kit[kb][:].rearrange("p (r k) -> p r k", r=4),
                  KITall[:, :, ksl].rearrange("r p k -> p r k"), writes=[b_kit[kb]])
            for h in range(8):
                pa = h % 3
                po = (h % 2) * 64
                s.op("pe", lambda e, h=h, pa=pa, po=po, kb=kb: e.matmul(
                    psA[pa][:], lhsT=qi_sb[po:po + 64, h // 2, :], rhs=kit[kb][po:po + 64, :],
                    start=True, stop=True), reads=[b_qi, b_kit[kb]], writes=[b_psA[pa]])
                s.op("act", lambda e, h=h, pa=pa: e.activation(
                    out=rr[h][:], in_=psA[pa][:], func=AF.Relu),
                    reads=[b_psA[pa]], writes=[b_rr[h]])
            for h in range(8):
                s.op("pe", lambda e, h=h: e.matmul(
                    psI[:], lhsT=diagw[:, h, :], rhs=rr[h][:], start=(h == 0), stop=(h == 7)),
                    reads=[b_dg, b_rr[h]], writes=[b_psI])
            dst = I_sb[:, kc * 512:(kc + 1) * 512]
            if ev % 2 == 0:
                s.op("dve", lambda e, dst=dst: e.tensor_copy(out=dst, in_=psI[:]),
                     reads=[b_psI], writes=[b_I])
            else:
                s.op("act", lambda e, dst=dst: e.copy(out=dst, in_=psI[:]),
                     reads=[b_psI], writes=[b_I])
            ev += 1
        Iv = I_sb[:, 0:nk]
        s.op("dve", lambda e, Iv=Iv: e.tensor_reduce(
            out=Mx[:], in_=Iv, axis=AX.X, op=ALU.max, apply_absolute_value=True),
            reads=[b_I], writes=[b_Mx])
        last = I_sb[:, nk - 512:nk]
        s.op("dve", lambda e, last=last: e.tensor_tensor(
            out=last, in0=last, in1=cb_sb[:], op=ALU.add), reads=[b_I, b_cb], writes=[b_I])
        s.op("dve", lambda e: e.tensor_scalar(
            out=Mx[:], in0=Mx[:], scalar1=1.001, scalar2=1e-20, op0=ALU.mult, op1=ALU.add),
            reads=[b_Mx], writes=[b_Mx])
        s.op("dve", lambda e: e.tensor_scalar(
            out=Mi[:], in0=pw2[:], scalar1=Mx[:, 0:1], scalar2=None, op0=ALU.mult),
            reads=[b_Mx, b_pw2], writes=[b_Mi])
        s.op("dve", lambda e: e.memset(cand[:], 0.0), writes=[b_cand])
        for it in range(nbis):
            s.op("dve", lambda e, Iv=Iv, nk=nk: e.tensor_scalar(
                out=junk[:, 0:nk], in0=Iv, scalar1=cand[:, 0:1], scalar2=None,
                op0=ALU.is_ge, op1=ALU.add, accum_out=cnt[:]),
                reads=[b_I, b_cand], writes=[b_junk, b_cnt])
            lastit = it == nbis - 1
            s.op("dve", lambda e, lastit=lastit: e.tensor_scalar(
                out=dd[:], in0=cnt[:], scalar1=TOPK - 0.5, scalar2=(1.0 if lastit else 0.5),
                op0=ALU.is_ge, op1=ALU.subtract), reads=[b_cnt], writes=[b_dd])
            dst_t, dst_b = (thr, b_thr) if lastit else (cand, b_cand)
            s.op("dve", lambda e, it=it, dst_t=dst_t: e.scalar_tensor_tensor(
                out=dst_t[:], in0=dd[:], scalar=Mi[:, it:it + 1], in1=cand[:],
                op0=ALU.mult, op1=ALU.add), reads=[b_dd, b_Mi, b_cand], writes=[dst_b])
        nkt = 4 * nkc
        for kc in range(nkc):
            ksl = slice(kc * 128, (kc + 1) * 128)
            kb = kc % 2
            for r4 in range(4):
                s.dma("sp", ch_kt[kb], kt[kb][:, :, r4 * 128:(r4 + 1) * 128],
                      KTall[r4, :, :, ksl], writes=[b_kt[kb]])
            s.dma("pool", ch_vv[kb], vv[kb][:], Vall[:, ksl, :].rearrange("r p f -> p r f"),
                  writes=[b_vv[kb]])
            s.op("dve", lambda e, kc=kc: e.tensor_scalar(
                out=mk[:], in0=I_sb[:, kc * 512:(kc + 1) * 512], scalar1=thr[:, 0:1],
                scalar2=None, op0=ALU.is_ge), reads=[b_I, b_thr], writes=[b_mk])
            for r4 in range(4):
                s.op("pe", lambda e, r4=r4: e.transpose(
                    out=psMT[:, r4 * 128:(r4 + 1) * 128], in_=mk[:, r4 * 128:(r4 + 1) * 128],
                    identity=idb[:]), reads=[b_mk, b_idb], writes=[b_psMT])
            s.op("act", lambda e: e.copy(out=mT[:].rearrange("p r q -> p (r q)"),
                                         in_=psMT[:, 0:512]),
                 reads=[b_psMT], writes=[b_mT])
            for r4 in range(4):
                ktile = 4 * kc + r4
                rel = 4 * (kc - i) + r4
                eb = ktile % 2
                for hg in range(2):
                    for hh in range(4):
                        h = hg * 4 + hh
                        hasb = rel >= -1
                        s.op("pe", lambda e, h=h, hg=hg, hh=hh, kb=kb, r4=r4, hasb=hasb: e.matmul(
                            psA[hg][:, hh * 128:(hh + 1) * 128],
                            lhsT=kt[kb][:, h, r4 * 128:(r4 + 1) * 128], rhs=q_sb[:, h, :],
                            start=(hh == 0), stop=(hh == 3 and not hasb), skip_group_check=True),
                            reads=[b_kt[kb], b_q], writes=[b_psA[hg]])
                        if hasb:
                            s.op("pe", lambda e, h=h, hg=hg, hh=hh, rel=rel: e.matmul(
                                psA[hg][:, hh * 128:(hh + 1) * 128],
                                lhsT=idb[:], rhs=tb_sb[:, rel + 1, h, :],
                                start=False, stop=(hh == 3), skip_group_check=True),
                                reads=[b_tb, b_idb], writes=[b_psA[hg]])
                    s.op("act", lambda e, hg=hg, eb=eb: e.activation(
                        out=E[eb][:, hg * 4:(hg + 1) * 4, :].rearrange("p h q -> p (h q)"),
                        in_=psA[hg][:], func=AF.Exp, scale=float(SCALE)),
                        reads=[b_psA[hg]], writes=[b_E[eb][hg]])
                s.op("dve", lambda e, eb=eb, r4=r4: e.tensor_tensor(
                    out=P[eb][:], in0=E[eb][:],
                    in1=mT[:, r4, :].unsqueeze(1).to_broadcast([128, 8, 128]), op=ALU.mult),
                    reads=[b_E[eb][0], b_E[eb][1], b_mT], writes=[b_P[eb]])
                for bk, (h0, h1) in enumerate(hb):
                    for h in range(h0, h1):
                        hh = h - h0
                        s.op("pe", lambda e, h=h, hh=hh, bk=bk, eb=eb, kb=kb, r4=r4,
                             ktile=ktile, h0=h0, h1=h1: e.matmul(
                            psO[bk][:, hh * 129:(hh + 1) * 129],
                            lhsT=P[eb][:, h, :], rhs=vv[kb][:, r4, h * 129:(h + 1) * 129],
                            start=(ktile == 0 and h == h0),
                            stop=(ktile == nkt - 1 and h == h1 - 1), skip_group_check=True),
                            reads=[b_P[eb], b_vv[kb]], writes=[b_psO[bk]])
        for bk, (h0, h1) in enumerate(hb):
            nh = h1 - h0
            ov = psO[bk][:, 0:nh * 129].rearrange("p (h f) -> p h f", f=129)
            s.op("dve", lambda e, ov=ov, h0=h0, h1=h1: e.reciprocal(
                out=rec[:, h0:h1], in_=ov[:, :, 128]), reads=[b_psO[bk]], writes=[b_rec])
            s.op("dve", lambda e, ov=ov, h0=h0, h1=h1, nh=nh: e.tensor_tensor(
                out=y_sb[:, h0:h1, :], in0=ov[:, :, 0:128],
                in1=rec[:, h0:h1].unsqueeze(2).to_broadcast([128, nh, 128]), op=ALU.mult),
                reads=[b_psO[bk], b_rec], writes=[b_y])
        for h in range(8):
            pa = h // 4
            s.op("pe", lambda e, h=h, pa=pa: e.transpose(
                out=psA[pa][:, (h % 4) * 128:(h % 4 + 1) * 128], in_=y_sb[:, h, :],
                identity=idf[:]), reads=[b_y, b_idf], writes=[b_psA[pa]])
        for pa in range(2):
            s.op("act", lambda e, pa=pa: e.copy(
                out=yT[:, pa * 4:(pa + 1) * 4, :].rearrange("p h q -> p (h q)"), in_=psA[pa][:]),
                reads=[b_psA[pa]], writes=[b_yT])
        s.dma("sp", ch_out, YAT[:, :, tsl], yT[:], reads=[b_yT], writes=[b_out])
    if not standalone:
        return
    s.wait_all("sp", [b_out])
    return c.finish()


def t5_bucket_np(d):
    d = np.maximum(d, 0).astype(np.int64)
    df = np.maximum(d, 1).astype(np.float32)
    large = 16 + (np.log(df / np.float32(16.0)) / np.float32(np.log(8.0))
                  * np.float32(16.0)).astype(np.int32)
    large = np.minimum(large, 31)
    return np.where(d < 16, d, large).astype(np.int64)


def host_consts_C(rel_bias, r):
    k = np.arange(128)[:, None, None]
    rel = (np.arange(5) - 1)[None, :, None]
    q = np.arange(128)[None, None, :]
    d = (r - rel) * 128 + q - k
    bidx = np.where(d >= 0, t5_bucket_np(d), 31)
    tbg = rel_bias[bidx]
    tbg = np.ascontiguousarray(np.transpose(tbg, (0, 1, 3, 2))).astype(np.float32)
    chh = np.ascontiguousarray(np.broadcast_to(rel_bias[31][None, :], (128, 8))).astype(np.float32)
    qq = np.arange(128)[:, None]
    kk = np.arange(512)[None, :]
    causal = (kk // 128 < r) | ((kk // 128 == r) & (kk % 128 <= qq))
    cb = np.where(causal, 0.0, -1e30).astype(np.float32)
    return tbg, chh, cb


GT = 3
NG = NSLOT // GT
GW = GT * 128


def rms_prep(c, s, h_sb, b_h, junk, b_junk, ss, b_ss, xs, b_xs, scal):
    ms, b_ms, rstd, b_rstd = scal
    s.op("act", lambda e: e.activation(out=junk[:], in_=h_sb[:], func=AF.Square,
                                       accum_out=ss[:]), reads=[b_h], writes=[b_junk, b_ss])
    s.op("dve", lambda e: e.tensor_scalar(out=ms[:], in0=ss[:], scalar1=1.0 / D, scalar2=EPS,
                                          op0=ALU.mult, op1=ALU.add), reads=[b_ss], writes=[b_ms])
    s.op("act", lambda e: e.activation(out=ms[:], in_=ms[:], func=AF.Sqrt),
         reads=[b_ms], writes=[b_ms])
    s.op("dve", lambda e: e.reciprocal(out=rstd[:], in_=ms[:]), reads=[b_ms], writes=[b_rstd])
    s.op("dve", lambda e: e.tensor_scalar(out=xs[:], in0=h_sb[:], scalar1=rstd[:, 0:1],
                                          scalar2=None, op0=ALU.mult),
         reads=[b_h, b_rstd], writes=[b_xs])


def load_weight_bf16(c, s, dst, b_dst, W, rows, cols, gcol, b_g, stg, b_stg, ch_stg, cw, cnt0=0,
                     extra=None):
    n = cnt0
    for kc in range(rows // 128):
        for c0 in range(0, cols, cw):
            c1 = min(cols, c0 + cw)
            sb_ = n % 2
            n += 1
            s.dma("sp", ch_stg[sb_], stg[sb_][:, 0:c1 - c0], W[kc * 128:(kc + 1) * 128, c0:c1],
                  writes=[b_stg[sb_]])
            if gcol is not None:
                s.op("dve", lambda e, kc=kc, c0=c0, c1=c1, sb_=sb_: e.tensor_scalar(
                    out=dst[:, kc, c0:c1], in0=stg[sb_][:, 0:c1 - c0], scalar1=gcol[:, kc:kc + 1],
                    scalar2=None, op0=ALU.mult), reads=[b_stg[sb_], b_g], writes=[b_dst])
            else:
                s.op("dve", lambda e, kc=kc, c0=c0, c1=c1, sb_=sb_: e.tensor_copy(
                    out=dst[:, kc, c0:c1], in_=stg[sb_][:, 0:c1 - c0]),
                    reads=[b_stg[sb_]], writes=[b_dst])
            if extra is not None:
                extra(kc, c0, c1, stg[sb_], b_stg[sb_])
    return n


def build_A(c=None, T=None):
    standalone = c is None
    if standalone:
        c = Ctx()
    s = c.s
    HIN = c.io(T, "HIN", [TOK, D], F32, "ExternalInput")
    WIN = c.io(T, "WIN", [D, NIN], F32, "ExternalInput")
    G1 = c.io(T, "G1", [D], F32, "ExternalInput")
    QT = c.io(T, "QT", [128, 8, TOK], BF16, "ExternalOutput")
    KT = c.io(T, "KT", [128, 8, TOK], BF16, "ExternalOutput")
    V = c.io(T, "V", [TOK, 1032], BF16, "ExternalOutput")
    QIT = c.io(T, "QIT", [128, 4, TOK], BF16, "ExternalOutput")
    KIT = c.io(T, "KIT", [128, TOK], BF16, "ExternalOutput")
    WI = c.io(T, "WI", [TOK, 8], F32, "ExternalOutput")
    URT = c.io(T, "URT", [128, 8, TOK], F32, "ExternalOutput")
    GUT = c.io(T, "GUT", [128, 8, TOK], F32, "ExternalOutput")
    SGR = c.io(T, "SGR", [128, 8, TOK], F32, "ExternalOutput")
    SGA = c.io(T, "SGA", [128, 8, TOK], F32, "ExternalOutput")

    idb, b_idb, idf, b_idf = const_identities(c)
    Wb = c.sb([128, 8, NIN], BF16); b_Wb = Buf()
    Wki = c.sb([128, 8, 128], BF16); b_Wki = Buf()
    CWS = 1938
    stg = [c.sb([128, CWS], F32) for _ in range(2)]; b_stg = [Buf(), Buf()]
    ch_stg = [Chan(), Chan()]
    g_sb = c.sb([128, 8], F32); b_g = Buf()
    ch_misc = Chan()
    s.dma("sp", ch_misc, g_sb[:], G1.rearrange("(c p) -> p c", p=128), writes=[b_g],
          allow_slow_non_contiguous=True)

    def extra(kc, c0, c1, st, b_st):
        if c0 <= 5632 and c1 >= 5696:
            o = 5632 - c0
            for hf in range(2):
                s.op("dve", lambda e, kc=kc, o=o, hf=hf, st=st: e.tensor_scalar(
                    out=Wki[:, kc, hf * 64:(hf + 1) * 64], in0=st[:, o:o + 64],
                    scalar1=g_sb[:, kc:kc + 1], scalar2=None, op0=ALU.mult),
                    reads=[b_st, b_g], writes=[b_Wki])

    load_weight_bf16(c, s, Wb, b_Wb, WIN, D, NIN, g_sb, b_g, stg, b_stg, ch_stg, CWS, extra=extra)

    h_sb = [c.sb([128, D], F32) for _ in range(2)]; b_h = [Buf(), Buf()]
    ch_h = [Chan(), Chan()]
    junk = c.sb([128, D], BF16); b_junk = Buf()
    xs = c.sb([128, D], BF16); b_xs = Buf()
    ss = c.sb([128, 1], F32); b_ss = Buf()
    ms = c.sb([128, 1], F32); b_ms = Buf()
    rstd = c.sb([128, 1], F32); b_rstd = Buf()
    xnT = [c.sb([128, 8, GW], BF16) for _ in range(2)]; b_xnT = [Buf(), Buf()]
    v_sb = [c.sb([128, 8, 129], BF16) for _ in range(2)]; b_v = [Buf(), Buf()]
    ch_v = [Chan(), Chan()]
    wi_sb = [c.sb([128, 8], F32) for _ in range(2)]; b_wi = [Buf(), Buf()]
    ch_wi = [Chan(), Chan()]
    NO = 4
    of32 = [c.sb([128, GW], F32) for _ in range(NO)]; b_of = [Buf() for _ in range(NO)]
    ch_of = [Chan() for _ in range(NO)]
    obf = [c.sb([128, GW], BF16) for _ in range(NO)]; b_ob = [Buf() for _ in range(NO)]
    ch_ob = [Chan() for _ in range(NO)]
    psT = c.ps([128, 1024], BF16); b_psT = Buf()
    psV = [c.ps([128, 512], F32) for _ in range(2)]; b_psV = [Buf(), Buf()]
    psW = c.ps([128, 512], F32); b_psW = Buf()
    psF = [c.ps([128, 512], F32) for _ in range(3)]; b_psF = [Buf() for _ in range(3)]
    b_dout = Buf()
    for vb in range(2):
        s.op("pool", lambda e, vb=vb: e.memset(v_sb[vb][:], 1.0), writes=[b_v[vb]])

    chunks = []
    for ck in range(8):
        chunks.append(("gelu", Wb, b_Wb, ck * 128, GUT, ck))
    for ck in range(8):
        chunks.append(("copyf", Wb, b_Wb, 1024 + ck * 128, URT, ck))
    for ck in range(8):
        chunks.append(("copyb", Wb, b_Wb, 2048 + ck * 128, QT, ck))
    for ck in range(8):
        chunks.append(("copyb", Wb, b_Wb, 3072 + ck * 128, KT, ck))
    for ck in range(4):
        chunks.append(("copyb", Wb, b_Wb, 5120 + ck * 128, QIT, ck))
    chunks.append(("copyb", Wki, b_Wki, 0, KIT, None))
    for ck in range(8):
        chunks.append(("sig", Wb, b_Wb, 5704 + ck * 128, SGR, ck))
    for ck in range(8):
        chunks.append(("sig", Wb, b_Wb, 6728 + ck * 128, SGA, ck))

    nt = 0
    nf = 0
    nob = 0
    nof = 0
    cpy = 0
    for g in range(NG):
        gb = g % 2
        gsl = slice(g * GW, (g + 1) * GW)
        for tl in range(GT):
            t = g * GT + tl
            tb = nt % 2
            nt += 1
            tsl = slice(t * 128, (t + 1) * 128)
            s.dma("sp", ch_h[tb], h_sb[tb][:], HIN[tsl, :], writes=[b_h[tb]])
            rms_prep(c, s, h_sb[tb], b_h[tb], junk, b_junk, ss, b_ss, xs, b_xs,
                     (ms, b_ms, rstd, b_rstd))
            for kc in range(8):
                s.op("pe", lambda e, kc=kc: e.transpose(
                    out=psT[:, kc * 128:(kc + 1) * 128], in_=xs[:, kc * 128:(kc + 1) * 128],
                    identity=idb[:]), reads=[b_xs, b_idb], writes=[b_psT])
            s.op("act", lambda e, gb=gb, tl=tl: e.copy(
                out=xnT[gb][:, :, tl * 128:(tl + 1) * 128],
                in_=psT[:].rearrange("p (c t) -> p c t", c=8)),
                reads=[b_psT], writes=[b_xnT[gb]])
            for half in range(2):
                for kc in range(8):
                    s.op("pe", lambda e, kc=kc, half=half, gb=gb, tl=tl: e.matmul(
                        psV[half][:], lhsT=xnT[gb][:, kc, tl * 128:(tl + 1) * 128],
                        rhs=Wb[:, kc, 4096 + half * 512:4096 + (half + 1) * 512],
                        start=(kc == 0), stop=(kc == 7)),
                        reads=[b_xnT[gb], b_Wb], writes=[b_psV[half]])
                s.op("act", lambda e, half=half, tb=tb: e.copy(
                    out=v_sb[tb][:, half * 4:(half + 1) * 4, 0:128],
                    in_=psV[half][:].rearrange("p (h f) -> p h f", h=4)),
                    reads=[b_psV[half]], writes=[b_v[tb]])
            for kc in range(8):
                s.op("pe", lambda e, kc=kc, gb=gb, tl=tl: e.matmul(
                    psW[:, 0:8], lhsT=xnT[gb][:, kc, tl * 128:(tl + 1) * 128],
                    rhs=Wb[:, kc, 5696:5704], start=(kc == 0), stop=(kc == 7)),
                    reads=[b_xnT[gb], b_Wb], writes=[b_psW])
            s.op("dve", lambda e, tb=tb: e.tensor_copy(out=wi_sb[tb][:], in_=psW[:, 0:8]),
                 reads=[b_psW], writes=[b_wi[tb]])
            vout = T["V_out"](t) if (T is not None and "V_out" in T) else V[tsl, :]
            s.dma("pool", ch_v[tb], vout, v_sb[tb][:].rearrange("p h f -> p (h f)"),
                  reads=[b_v[tb]])
            s.dma("pool", ch_wi[tb], WI[tsl, :], wi_sb[tb][:], reads=[b_wi[tb]])
        for kind, Wt, b_Wt, col0, OUT, ck in chunks:
            pf = nf % 3
            nf += 1
            for kc in range(8):
                s.op("pe", lambda e, kc=kc, pf=pf, Wt=Wt, col0=col0, gb=gb: e.matmul(
                    psF[pf][:, 0:GW], lhsT=Wt[:, kc, col0:col0 + 128], rhs=xnT[gb][:, kc, :],
                    start=(kc == 0), stop=(kc == 7)),
                    reads=[b_Wt, b_xnT[gb]], writes=[b_psF[pf]])
            if T is not None and OUT is KT and "KT_out" in T:
                oap = T["KT_out"](g, ck)
            elif T is not None and OUT is URT and "URT_out" in T:
                oap = T["URT_out"](g, ck)
            elif T is not None and OUT is KIT and "KIT_out" in T:
                oap = T["KIT_out"](g)
            else:
                oap = OUT[:, gsl] if ck is None else OUT[:, ck, gsl]
            if kind == "copyb":
                ob = nob % NO
                nob += 1
                s.op("dve", lambda e, ob=ob, pf=pf: e.tensor_copy(out=obf[ob][:], in_=psF[pf][:, 0:GW]),
                     reads=[b_psF[pf]], writes=[b_ob[ob]])
                s.dma("pool", ch_ob[ob], oap, obf[ob][:], reads=[b_ob[ob]])
            else:
                of = nof % NO
                nof += 1
                if kind == "copyf":
                    s.op("dve", lambda e, of=of, pf=pf: e.tensor_copy(out=of32[of][:], in_=psF[pf][:, 0:GW]),
                         reads=[b_psF[pf]], writes=[b_of[of]])
                else:
                    fn = AF.Gelu_apprx_tanh if kind == "gelu" else AF.Sigmoid
                    s.op("act", lambda e, of=of, pf=pf, fn=fn: e.activation(
                        out=of32[of][:], in_=psF[pf][:, 0:GW], func=fn),
                        reads=[b_psF[pf]], writes=[b_of[of]])
                s.dma("sp", ch_of[of], oap, of32[of][:], reads=[b_of[of]])
        if T is not None and "after_group" in T:
            T["after_group"](g, ch_v + ch_wi + ch_ob + ch_of)
    if not standalone:
        return
    s.wait_chans("sp", ch_v + ch_wi + ch_ob + ch_of)
    return c.finish()


def to_slots(a):
    sh = a.shape[1:]
    v = a.reshape(NSLOT, 4, 128, *sh)
    return [np.ascontiguousarray(v[:, r].reshape(TOK, *sh)) for r in range(4)]


def from_slots(parts):
    sh = parts[0].shape[1:]
    v = np.stack([p.reshape(NSLOT, 128, *sh) for p in parts], axis=1)
    return v.reshape(LP, *sh)


NCH = LP // 512


def build_B(c=None, T=None):
    standalone = c is None
    if standalone:
        c = Ctx()
    s = c.s
    URall = c.io(T, "URall", [4, 128, 8, TOK], F32, "ExternalInput")
    CW = c.io(T, "CW", [128, 8, 4], F32, "ExternalInput")
    CBI = c.io(T, "CBI", [128, 8], F32, "ExternalInput")
    WA = c.io(T, "WA", [4, 256, 256], F32, "ExternalInput")
    WX = c.io(T, "WX", [4, 256, 256], F32, "ExternalInput")
    BA = c.io(T, "BA", [128, 8], F32, "ExternalInput")
    BX = c.io(T, "BX", [128, 8], F32, "ExternalInput")
    LAM = c.io(T, "LAM", [128, 8], F32, "ExternalInput")
    HST = c.io(T, "HST", [128, 8, LP], F32, "ExternalOutput")

    ch_misc = Chan()
    cw = c.sb([128, 8, 4], F32); b_cw = Buf()
    cbi = c.sb([128, 8], F32); b_cbi = Buf()
    ba = c.sb([128, 8], F32); b_ba = Buf()
    bx = c.sb([128, 8], F32); b_bx = Buf()
    lam = c.sb([128, 8], F32); b_lam = Buf()
    one = c.sb([128, 1], F32); b_one = Buf()
    c1 = c.sb([128, 8], F32); b_c1 = Buf()
    c2 = c.sb([128, 8], F32); b_c2 = Buf()
    for t, src, b in ((cw, CW, b_cw), (cbi, CBI, b_cbi), (ba, BA, b_ba), (bx, BX, b_bx),
                      (lam, LAM, b_lam)):
        s.dma("sp", ch_misc, t[:], src, writes=[b])
    s.op("pool", lambda e: e.memset(one[:], 1.0), writes=[b_one])
    s.op("act", lambda e: e.activation(out=c1[:], in_=lam[:], func=AF.Exp, scale=-1.0),
         reads=[b_lam], writes=[b_c1])
    s.op("act", lambda e: e.activation(out=c1[:], in_=c1[:], func=AF.Ln, bias=one[:, 0:1]),
         reads=[b_c1, b_one], writes=[b_c1])
    s.op("dve", lambda e: e.tensor_scalar(out=c2[:], in0=c1[:], scalar1=-16.0, scalar2=None,
                                          op0=ALU.mult), reads=[b_c1], writes=[b_c2])
    s.op("dve", lambda e: e.tensor_scalar(out=c1[:], in0=c1[:], scalar1=-8.0, scalar2=None,
                                          op0=ALU.mult), reads=[b_c1, b_c2], writes=[b_c1])
    wa = c.sb([128, 2, 256], BF16); b_wa = Buf()
    wx = c.sb([128, 2, 256], BF16); b_wx = Buf()
    stg = [c.sb([128, 256], F32) for _ in range(2)]; b_stg = [Buf(), Buf()]
    ch_stg = [Chan(), Chan()]

    NBUF = 2
    u_sb = [[c.sb([128, 515], F32) for _ in range(NBUF)] for _ in range(2)]
    b_u = [[Buf() for _ in range(NBUF)] for _ in range(2)]
    ch_u = [[Chan() for _ in range(NBUF)] for _ in range(2)]
    xc = [[c.sb([128, 512], F32) for _ in range(NBUF)] for _ in range(2)]
    b_xc = [[Buf() for _ in range(NBUF)] for _ in range(2)]
    xcb = [[c.sb([128, 512], BF16) for _ in range(NBUF)] for _ in range(2)]
    b_xcb = [[Buf() for _ in range(NBUF)] for _ in range(2)]
    mk2 = lambda: [[c.sb([128, 512], F32) for _ in range(NBUF)] for _ in range(2)]
    mb2 = lambda: [[Buf() for _ in range(NBUF)] for _ in range(2)]
    rg_, b_rg_ = mk2(), mb2()
    ig_a, b_ig_a = mk2(), mb2()
    av_, b_av_ = mk2(), mb2()
    a2_, b_a2_ = mk2(), mb2()
    hs = [[c.sb([128, 512], F32) for _ in range(NBUF)] for _ in range(2)]
    b_hs = [[Buf() for _ in range(NBUF)] for _ in range(2)]
    ch_hs = [[Chan() for _ in range(NBUF)] for _ in range(2)]
    psa = [c.ps([128, 512], F32) for _ in range(2)]; b_psa = [Buf(), Buf()]
    psx = [c.ps([128, 512], F32) for _ in range(2)]; b_psx = [Buf(), Buf()]

    nw = 0
    for g in range(4):
        nw = load_weight_bf16(c, s, wa, b_wa, WA[g], 256, 256, None, None, stg, b_stg, ch_stg, 256,
                              cnt0=nw)
        nw = load_weight_bf16(c, s, wx, b_wx, WX[g], 256, 256, None, None, stg, b_stg, ch_stg, 256,
                              cnt0=nw)
        for cg in range(2):
            s.op("pool", lambda e, cg=cg: e.memset(u_sb[cg][NBUF - 1][:, 512:515], 0.0),
                 writes=[b_u[cg][NBUF - 1]])
        for kc in range(NCH):
            bb = kc % NBUF
            pb = (kc - 1) % NBUF
            ksl = slice(kc * 128, (kc + 1) * 128)
            for cg in range(2):
                ck = 2 * g + cg
                u = u_sb[cg][bb]
                s.op("pool", lambda e, u=u, cg=cg, pb=pb: e.tensor_copy(
                    out=u[:, 0:3], in_=u_sb[cg][pb][:, 512:515]),
                    reads=[b_u[cg][pb]], writes=[b_u[cg][bb]])
                uin = (T["UR_in"](ck, kc) if (T is not None and "UR_in" in T)
                       else URall[:, :, ck, ksl].rearrange("r p k -> p r k"))
                s.dma("sp", ch_u[cg][bb], u[:, 3:515].rearrange("p (r k) -> p r k", r=4),
                      uin, writes=[b_u[cg][bb]])
            for j in range(4):
                for cg in range(2):
                    ck = 2 * g + cg
                    u = u_sb[cg][bb]
                    x_ = xc[cg][bb]
                    if j == 0:
                        s.op("dve", lambda e, u=u, x_=x_, ck=ck: e.tensor_scalar(
                            out=x_[:], in0=u[:, 0:512], scalar1=cw[:, ck, 0:1],
                            scalar2=cbi[:, ck:ck + 1], op0=ALU.mult, op1=ALU.add),
                            reads=[b_u[cg][bb], b_cw, b_cbi], writes=[b_xc[cg][bb]])
                    else:
                        s.op("dve", lambda e, u=u, x_=x_, ck=ck, j=j: e.scalar_tensor_tensor(
                            out=x_[:], in0=u[:, j:j + 512], scalar=cw[:, ck, j:j + 1], in1=x_[:],
                            op0=ALU.mult, op1=ALU.add), reads=[b_u[cg][bb], b_cw, b_xc[cg][bb]],
                            writes=[b_xc[cg][bb]])
            for cg in range(2):
                x_ = xc[cg][bb]
                s.op("act", lambda e, x_=x_, cg=cg, bb=bb: e.copy(out=xcb[cg][bb][:], in_=x_[:]),
                     reads=[b_xc[cg][bb]], writes=[b_xcb[cg][bb]])
            rg = [rg_[0][bb], rg_[1][bb]]; b_rg = [b_rg_[0][bb], b_rg_[1][bb]]
            ig = [ig_a[0][bb], ig_a[1][bb]]; b_ig = [b_ig_a[0][bb], b_ig_a[1][bb]]
            av = [av_[0][bb], av_[1][bb]]; b_av = [b_av_[0][bb], b_av_[1][bb]]
            a2 = [a2_[0][bb], a2_[1][bb]]; b_a2 = [b_a2_[0][bb], b_a2_[1][bb]]
            for jg in range(2):
                for (wt, b_wt, ps_, b_ps) in ((wa, b_wa, psa[jg], b_psa[jg]),
                                              (wx, b_wx, psx[jg], b_psx[jg])):
                    for ig_ in range(2):
                        s.op("pe", lambda e, wt=wt, ps_=ps_, ig_=ig_, jg=jg, bb=bb: e.matmul(
                            ps_[:], lhsT=wt[:, ig_, jg * 128:(jg + 1) * 128], rhs=xcb[ig_][bb][:],
                            start=(ig_ == 0), stop=(ig_ == 1)),
                            reads=[b_wt, b_xcb[ig_][bb]], writes=[b_ps])
            for jg in range(2):
                ck = 2 * g + jg
                s.op("act", lambda e, jg=jg, ck=ck, rg=rg: e.activation(
                    out=rg[jg][:], in_=psa[jg][:], func=AF.Sigmoid, bias=ba[:, ck:ck + 1]),
                    reads=[b_psa[jg], b_ba], writes=[b_rg[jg]])
                s.op("act", lambda e, jg=jg, ck=ck, ig=ig: e.activation(
                    out=ig[jg][:], in_=psx[jg][:], func=AF.Sigmoid, bias=bx[:, ck:ck + 1]),
                    reads=[b_psx[jg], b_bx], writes=[b_ig[jg]])
            for jg in range(2):
                ck = 2 * g + jg
                s.op("act", lambda e, jg=jg, ck=ck, av=av, rg=rg: e.activation(
                    out=av[jg][:], in_=rg[jg][:], func=AF.Exp, scale=c1[:, ck:ck + 1]),
                    reads=[b_rg[jg], b_c1], writes=[b_av[jg]])
                s.op("act", lambda e, jg=jg, ck=ck, a2=a2, rg=rg: e.activation(
                    out=a2[jg][:], in_=rg[jg][:], func=AF.Exp, scale=c2[:, ck:ck + 1]),
                    reads=[b_rg[jg], b_c2], writes=[b_a2[jg]])
            for jg in range(2):
                s.op("act", lambda e, jg=jg, a2=a2: e.activation(
                    out=a2[jg][:], in_=a2[jg][:], func=AF.Sqrt, scale=-1.0, bias=one[:, 0:1]),
                    reads=[b_a2[jg], b_one], writes=[b_a2[jg]])
            for jg in range(2):
                s.op("dve", lambda e, jg=jg, bb=bb, ig=ig: e.tensor_tensor(
                    out=ig[jg][:], in0=ig[jg][:], in1=xc[jg][bb][:], op=ALU.mult),
                    reads=[b_ig[jg], b_xc[jg][bb]], writes=[b_ig[jg]])
            for jg in range(2):
                s.op("dve", lambda e, jg=jg, ig=ig, a2=a2: e.tensor_tensor(
                    out=ig[jg][:], in0=ig[jg][:], in1=a2[jg][:], op=ALU.mult),
                    reads=[b_ig[jg], b_a2[jg]], writes=[b_ig[jg]])
            for jg in range(2):
                ck = 2 * g + jg
                init = 0.0 if kc == 0 else hs[jg][pb][:, 511:512]
                rd = [b_av[jg], b_ig[jg]] + ([] if kc == 0 else [b_hs[jg][pb]])
                s.op("dve", lambda e, jg=jg, bb=bb, init=init, av=av, ig=ig: e.tensor_tensor_scan(
                    out=hs[jg][bb][:], data0=av[jg][:], data1=ig[jg][:], initial=init,
                    op0=ALU.mult, op1=ALU.add), reads=rd, writes=[b_hs[jg][bb]])
                s.dma("pool", ch_hs[jg][bb], HST[:, ck, kc * 512:(kc + 1) * 512], hs[jg][bb][:],
                      reads=[b_hs[jg][bb]])
    if not standalone:
        return
    s.wait_chans("sp", [ch for l in ch_hs for ch in l])
    return c.finish()


def build_D(c=None, T=None):
    standalone = c is None
    if standalone:
        c = Ctx()
    s = c.s
    HIN = c.io(T, "HIN", [TOK, D], F32, "ExternalInput")
    HSM = c.io(T, "HSM", [128, 8, LP], F32, "ExternalInput")
    RM = c.io(T, "RM", [128, 4], F32, "ExternalInput")
    GUT = c.io(T, "GUT", [128, 8, TOK], F32, "ExternalInput")
    SGR = c.io(T, "SGR", [128, 8, TOK], F32, "ExternalInput")
    SGA = c.io(T, "SGA", [128, 8, TOK], F32, "ExternalInput")
    YAT = c.io(T, "YAT", [128, 8, TOK], F32, "ExternalInput")
    WOUT = c.io(T, "WOUT", [D, D], F32, "ExternalInput")
    G2 = c.io(T, "G2", [D], F32, "ExternalInput")
    W1 = c.io(T, "W1", [D, DFF], F32, "ExternalInput")
    W2 = c.io(T, "W2", [DFF, D], F32, "ExternalInput")
    FG = c.io(T, "FG", [128, D], F32, "ExternalInput")
    HOUT = c.io(T, "HOUT", [TOK, D], F32, "ExternalOutput")
    FOUT = c.io(T, "FOUT", [TOK, D], F32, "ExternalOutput")

    idb, b_idb, idf, b_idf = const_identities(c)
    ch_misc = Chan()
    g_sb = c.sb([128, 8], F32); b_g = Buf()
    s.dma("sp", ch_misc, g_sb[:], G2.rearrange("(c p) -> p c", p=128), writes=[b_g],
          allow_slow_non_contiguous=True)
    fg = c.sb([128, D], F32); b_fg = Buf()
    s.dma("sp", ch_misc, fg[:], FG[:, :], writes=[b_fg])
    Wo = c.sb([128, 8, D], BF16); b_Wo = Buf()
    W1b = c.sb([128, 8, DFF], BF16); b_W1 = Buf()
    W2b = c.sb([128, 32, D], BF16); b_W2 = Buf()
    stg = [c.sb([128, 1024], F32) for _ in range(2)]; b_stg = [Buf(), Buf()]
    ch_stg = [Chan(), Chan()]
    n = load_weight_bf16(c, s, Wo, b_Wo, WOUT, D, D, None, None, stg, b_stg, ch_stg, 1024)
    n = load_weight_bf16(c, s, W1b, b_W1, W1, D, DFF, g_sb, b_g, stg, b_stg, ch_stg, 1024, cnt0=n)
    load_weight_bf16(c, s, W2b, b_W2, W2, DFF, D, None, None, stg, b_stg, ch_stg, 1024, cnt0=n)

    rm = c.sb([128, 4], F32); b_rm = Buf()
    s.dma("sp", ch_misc, rm[:], RM[:, :], writes=[b_rm])
    hs4 = [c.sb([128, 512], F32) for _ in range(2)]; b_hs4 = [Buf(), Buf()]; ch_hs4 = [Chan(), Chan()]
    NI = 2
    names = ("hs", "gu", "sgr", "sga", "ya")
    srcs = (HSM, GUT, SGR, SGA, YAT)
    it = {nm: [c.sb([128, 128], F32) for _ in range(NI)] for nm in names}
    b_it = {nm: [Buf() for _ in range(NI)] for nm in names}
    ch_it = {nm: [Chan() for _ in range(NI)] for nm in names}
    yT = c.sb([128, 8, 128], BF16); b_yT = Buf()
    hres = c.sb([128, D], F32); b_hres = Buf(); ch_hres = Chan()
    h1 = c.sb([128, D], F32); b_h1 = Buf()
    h2 = [c.sb([128, D], F32)] * 2; b_h2 = [Buf()] * 2; ch_h2 = [Chan()] * 2
    fo = [c.sb([128, D], F32)] * 2; b_fo = [Buf()] * 2; ch_fo = [Chan()] * 2
    junk = c.sb([128, D], BF16); b_junk = Buf()
    xs = c.sb([128, D], BF16); b_xs = Buf()
    ss = c.sb([128, 1], F32); b_ss = Buf()
    ms = c.sb([128, 1], F32); b_ms = Buf()
    rstd = c.sb([128, 1], F32); b_rstd = Buf()
    hnT = c.sb([128, 8, 128], BF16); b_hnT = Buf()
    r32 = [c.sb([128, 512], F32) for _ in range(2)]; b_r32 = [Buf(), Buf()]
    AT = c.sb([128, 32, 128], BF16); b_AT = Buf()
    psH = [c.ps([128, 512], F32) for _ in range(2)]; b_psH = [Buf(), Buf()]
    psT = c.ps([128, 1024], BF16); b_psT = Buf()
    psF = [c.ps([128, 512], F32) for _ in range(2)]; b_psF = [Buf(), Buf()]
    psG = [c.ps([128, 512], F32) for _ in range(2)]; b_psG = [Buf(), Buf()]

    ni = 0
    for t in range(NSLOT):
        tsl = slice(t * 128, (t + 1) * 128)
        ob = t % 2
        s.dma("pool", ch_hres, hres[:], HIN[tsl, :], writes=[b_hres])
        for kc in range(8):
            ib = ni % NI
            ni += 1
            for nm, src in zip(names, srcs):
                if nm == "hs":
                    continue
                s.dma("sp", ch_it[nm][ib], it[nm][ib][:], src[:, kc, tsl], writes=[b_it[nm][ib]])
            a, b_a = it["hs"][ib], b_it["hs"][ib]
            y2, b_y2 = it["ya"][ib], b_it["ya"][ib]
            s.dma("sp", ch_hs4[ib], hs4[ib][:], HSM[:, kc, t * 512:(t + 1) * 512], writes=[b_hs4[ib]])
            s.op("dve", lambda e, a=a, ib=ib: e.tensor_scalar(
                out=a[:], in0=hs4[ib][:, 0:128], scalar1=rm[:, 0:1], scalar2=None, op0=ALU.mult),
                reads=[b_hs4[ib], b_rm], writes=[b_a])
            for r4 in range(1, 4):
                s.op("dve", lambda e, a=a, ib=ib, r4=r4: e.scalar_tensor_tensor(
                    out=a[:], in0=hs4[ib][:, r4 * 128:(r4 + 1) * 128], scalar=rm[:, r4:r4 + 1],
                    in1=a[:], op0=ALU.mult, op1=ALU.add), reads=[b_hs4[ib], b_rm, b_a], writes=[b_a])
            s.op("dve", lambda e, a=a, ib=ib: e.tensor_tensor(out=a[:], in0=a[:], in1=it["gu"][ib][:],
                                                              op=ALU.mult),
                 reads=[b_a, b_it["gu"][ib]], writes=[b_a])
            s.op("dve", lambda e, a=a, ib=ib: e.tensor_tensor(out=a[:], in0=a[:], in1=it["sgr"][ib][:],
                                                              op=ALU.mult),
                 reads=[b_a, b_it["sgr"][ib]], writes=[b_a])
            s.op("pool", lambda e, y2=y2, ib=ib: e.tensor_tensor(out=y2[:], in0=y2[:],
                                                                 in1=it["sga"][ib][:], op=ALU.mult),
                 reads=[b_y2, b_it["sga"][ib]], writes=[b_y2])
            s.op("dve", lambda e, a=a, y2=y2, kc=kc: e.tensor_tensor(out=yT[:, kc, :], in0=a[:],
                                                                     in1=y2[:], op=ALU.add),
                 reads=[b_a, b_y2], writes=[b_yT])
        for half in range(2):
            for kc in range(8):
                s.op("pe", lambda e, kc=kc, half=half: e.matmul(
                    psH[half][:], lhsT=yT[:, kc, :], rhs=Wo[:, kc, half * 512:(half + 1) * 512],
                    start=(kc == 0), stop=(kc == 7)), reads=[b_yT, b_Wo], writes=[b_psH[half]])
            s.op("dve", lambda e, half=half: e.tensor_tensor(
                out=h1[:, half * 512:(half + 1) * 512], in0=psH[half][:],
                in1=hres[:, half * 512:(half + 1) * 512], op=ALU.add),
                reads=[b_psH[half], b_hres], writes=[b_h1])
        rms_prep(c, s, h1, b_h1, junk, b_junk, ss, b_ss, xs, b_xs, (ms, b_ms, rstd, b_rstd))
        for kc in range(8):
            s.op("pe", lambda e, kc=kc: e.transpose(
                out=psT[:, kc * 128:(kc + 1) * 128], in_=xs[:, kc * 128:(kc + 1) * 128],
                identity=idb[:]), reads=[b_xs, b_idb], writes=[b_psT])
        s.op("act", lambda e: e.copy(out=hnT[:].rearrange("p c t -> p (c t)"), in_=psT[:]),
             reads=[b_psT], writes=[b_hnT])
        for f4 in range(8):
            pf = f4 % 2
            for fq in range(4):
                fc = f4 * 4 + fq
                for kc in range(8):
                    s.op("pe", lambda e, kc=kc, fc=fc, fq=fq, pf=pf: e.matmul(
                        psF[pf][:, fq * 128:(fq + 1) * 128], lhsT=W1b[:, kc, fc * 128:(fc + 1) * 128],
                        rhs=hnT[:, kc, :], start=(kc == 0 and fq == 0), stop=(kc == 7 and fq == 3),
                        skip_group_check=True), reads=[b_W1, b_hnT], writes=[b_psF[pf]])
            s.op("act", lambda e, pf=pf: e.activation(out=r32[pf][:], in_=psF[pf][:], func=AF.Relu),
                 reads=[b_psF[pf]], writes=[b_r32[pf]])
            s.op("dve", lambda e, pf=pf, f4=f4: e.tensor_tensor(
                out=AT[:, f4 * 4:(f4 + 1) * 4, :].rearrange("p f t -> p (f t)"), in0=r32[pf][:],
                in1=r32[pf][:], op=ALU.mult), reads=[b_r32[pf]], writes=[b_AT])
        for half in range(2):
            for fc in range(32):
                s.op("pe", lambda e, fc=fc, half=half: e.matmul(
                    psG[half][:], lhsT=AT[:, fc, :], rhs=W2b[:, fc, half * 512:(half + 1) * 512],
                    start=(fc == 0), stop=(fc == 31)), reads=[b_AT, b_W2], writes=[b_psG[half]])
            s.op("dve", lambda e, half=half, ob=ob: e.tensor_tensor(
                out=h2[ob][:, half * 512:(half + 1) * 512], in0=psG[half][:],
                in1=h1[:, half * 512:(half + 1) * 512], op=ALU.add),
                reads=[b_psG[half], b_h1], writes=[b_h2[ob]])
        s.dma("pool", ch_h2[ob], HOUT[tsl, :], h2[ob][:], reads=[b_h2[ob]])
        s.op("act", lambda e, ob=ob: e.activation(out=junk[:], in_=h2[ob][:], func=AF.Square,
                                                  accum_out=ss[:]),
             reads=[b_h2[ob]], writes=[b_junk, b_ss])
        s.op("dve", lambda e: e.tensor_scalar(out=ms[:], in0=ss[:], scalar1=1.0 / D, scalar2=EPS,
                                              op0=ALU.mult, op1=ALU.add), reads=[b_ss], writes=[b_ms])
        s.op("act", lambda e: e.activation(out=ms[:], in_=ms[:], func=AF.Sqrt),
             reads=[b_ms], writes=[b_ms])
        s.op("dve", lambda e: e.reciprocal(out=rstd[:], in_=ms[:]), reads=[b_ms], writes=[b_rstd])
        s.op("dve", lambda e, ob=ob: e.scalar_tensor_tensor(
            out=fo[ob][:], in0=h2[ob][:], scalar=rstd[:, 0:1], in1=fg[:], op0=ALU.mult,
            op1=ALU.mult), reads=[b_h2[ob], b_rstd, b_fg], writes=[b_fo[ob]])
        s.dma("pool", ch_fo[ob], FOUT[tsl, :], fo[ob][:], reads=[b_fo[ob]])
    if not standalone:
        return
    s.wait_chans("sp", ch_h2 + ch_fo)
    return c.finish()


def _chan_cols(vec, r):
    return np.ascontiguousarray(vec[r * 256:(r + 1) * 256].reshape(2, 128).T).astype(np.float32)


_DBG = None


def kernel_unfused(x, norm1_g, w_in, conv_w, conv_b, w_rg_a, b_rg_a, w_rg_x, b_rg_x, lru_lambda, w_out,
           norm2_g, w_mlp1, w_mlp2, rel_bias, meta_tokens, final_g):
    f32 = np.float32
    x = np.asarray(x, f32)
    rel_bias = np.asarray(rel_bias, f32)
    hs = []
    for b in range(NB):
        h = np.zeros((LP, D), f32)
        h[:NMETA] = np.asarray(meta_tokens, f32)
        h[NMETA:L] = x[b]
        hs.extend(to_slots(h))
    constsC = [host_consts_C(rel_bias, r) for r in range(4)]
    fgb = np.ascontiguousarray(np.broadcast_to(np.asarray(final_g, f32)[None, :], (128, D)))
    fout = None
    for l in range(2):
        wl = np.ascontiguousarray(np.asarray(w_in[l], f32))
        g1 = np.ascontiguousarray(np.asarray(norm1_g[l], f32))
        resA = run_spmd(build_A(), [dict(HIN=hs[c], WIN=wl, G1=g1) for c in range(NCORE)])
        in_maps = []
        cols8 = lambda v: np.ascontiguousarray(np.asarray(v, f32).reshape(8, 128).T)
        cw8 = np.ascontiguousarray(np.asarray(conv_w[l], f32).reshape(4, 8, 128).transpose(2, 1, 0))
        for c in range(NCORE):
            b, r = divmod(c, 4)
            in_maps.append(dict(
                URall=np.stack([resA[4 * b + rr]["URT"] for rr in range(4)], 0), CW=cw8,
                CBI=cols8(conv_b[l]), WA=np.ascontiguousarray(np.asarray(w_rg_a[l], f32)),
                WX=np.ascontiguousarray(np.asarray(w_rg_x[l], f32)), BA=cols8(b_rg_a[l]),
                BX=cols8(b_rg_x[l]), LAM=cols8(lru_lambda[l])))
        resB = run_spmd(build_B(), in_maps)
        in_maps = []
        for c in range(NCORE):
            b, r = divmod(c, 4)
            grp = [resA[4 * b + rr] for rr in range(4)]
            tbg, chh, cb = constsC[r]
            in_maps.append(dict(
                QT=resA[c]["QT"], QIT=resA[c]["QIT"], WI=resA[c]["WI"],
                KTall=np.stack([g["KT"] for g in grp], 0), Vall=np.stack([g["V"] for g in grp], 0),
                KITall=np.stack([g["KIT"] for g in grp], 0), CB=cb, TBG=tbg, CH=chh))
        resC = run_spmd(build_C(), in_maps)
        in_maps = []
        wo = np.ascontiguousarray(np.asarray(w_out[l], f32))
        g2 = np.ascontiguousarray(np.asarray(norm2_g[l], f32))
        w1 = np.ascontiguousarray(np.asarray(w_mlp1[l], f32))
        w2 = np.ascontiguousarray(np.asarray(w_mlp2[l], f32))
        for c in range(NCORE):
            b, r = divmod(c, 4)
            rmask = np.zeros((128, 4), f32)
            rmask[:, r] = 1.0
            in_maps.append(dict(
                HIN=hs[c], HSM=resB[c]["HST"], RM=rmask, GUT=resA[c]["GUT"], SGR=resA[c]["SGR"],
                SGA=resA[c]["SGA"], YAT=resC[c]["YAT"], WOUT=wo, G2=g2, W1=w1, W2=w2, FG=fgb))
        resD = run_spmd(build_D(), in_maps)
        if _DBG is not None:
            _DBG[l] = dict(A=resA, B=resB, C=resC, D=resD, hin=hs)
            if _DBG.get("stop") == l:
                return None
        hs = [resD[c]["HOUT"] for c in range(NCORE)]
        fout = [resD[c]["FOUT"] for c in range(NCORE)]
    out = np.zeros((NB, SEQ, D), f32)
    for b in range(NB):
        full = from_slots(fout[4 * b:4 * b + 4])
        out[b] = full[NMETA:L]
    return out


GROUPS = [[0, 1, 2, 3], [4, 5, 6, 7]]
CC_INC = 1


def build_fused(nlayer=2, CFN=None):
    CFN = CFN or build_C2
    c = Ctx()
    s = c.s
    nc = c.nc
    ext = lambda n, sh, dt: c.dram(n, sh, dt, "ExternalInput")
    HIN0 = ext("HIN0", [TOK, D], F32)
    WIN = ext("WIN", [2, D, NIN], F32)
    G1 = ext("G1", [2, D], F32)
    CW = ext("CW", [2, 128, 8, 4], F32)
    CBI = ext("CBI", [2, 128, 8], F32)
    WA = ext("WA", [2, 4, 256, 256], F32)
    WX = ext("WX", [2, 4, 256, 256], F32)
    BA = ext("BA", [2, 128, 8], F32)
    BX = ext("BX", [2, 128, 8], F32)
    LAM = ext("LAM", [2, 128, 8], F32)
    WOUT = ext("WOUT", [2, D, D], F32)
    G2 = ext("G2", [2, D], F32)
    W1 = ext("W1", [2, D, DFF], F32)
    W2 = ext("W2", [2, DFF, D], F32)
    FG = ext("FG", [128, D], F32)
    RM = ext("RM", [128, 4], F32)
    CB = ext("CB", [128, 512], F32)
    TBG = ext("TBG", [128, 5, 8, 128], F32)
    CH = ext("CH", [128, 8], F32)
    FOUT = c.dram("FOUT", [TOK, D], F32, "ExternalOutput")

    it = lambda n, sh, dt: nc.dram_tensor(n, list(sh), dt).ap()
    QT = it("iQT", [128, 8, TOK], BF16)
    QIT = it("iQIT", [128, 4, TOK], BF16)
    WI = it("iWI", [TOK, 8], F32)
    GUT = it("iGUT", [128, 8, TOK], F32)
    SGR = it("iSGR", [128, 8, TOK], F32)
    SGA = it("iSGA", [128, 8, TOK], F32)
    YAT = it("iYAT", [128, 8, TOK], F32)
    KTp = [it(f"iKT{g}", [1024, GW], BF16) for g in range(NG)]
    KTg = [it(f"iKTg{g}", [4096, GW], BF16) for g in range(NG)]
    Vp = [it(f"iV{g}", [GW, 1032], BF16) for g in range(NG)]
    Vg = [it(f"iVg{g}", [4 * GW, 1032], BF16) for g in range(NG)]
    KIp = [it(f"iKI{g}", [128, GW], BF16) for g in range(NG)]
    KIg = [it(f"iKIg{g}", [512, GW], BF16) for g in range(NG)]
    URp = [[it(f"iUR{g}_{hf}", [512, GW], F32) for hf in range(2)] for g in range(NG)]
    URg = [[it(f"iURg{g}_{hf}", [2048, GW], F32) for hf in range(2)] for g in range(NG)]
    dummyKT = it("iKTd", [128, 8, 128], BF16)
    dummyV = it("iVd", [128, 1032], BF16)
    dummyKI = it("iKId", [128, 128], BF16)
    dummyUR = it("iURd", [128, 8, 128], F32)

    def offs(kc):
        return kc // GT, (kc % GT) * 128
    HST = it("iHST", [128, 8, LP], F32)
    H1 = it("iH1", [TOK, D], F32)
    HX = it("iHX", [TOK, D], F32)
    FX = it("iFX", [TOK, D], F32)

    for l in range(nlayer):
        hin = HIN0 if l == 0 else H1
        cch = Chan()

        def after_group(g, chans, cch=cch):
            s.wait_chans("pool", chans)
            s.coll("AllGather", cch, [URp[g][0]], [URg[g][0]], GROUPS, CC_INC)
            s.coll("AllGather", cch, [URp[g][1]], [URg[g][1]], GROUPS, CC_INC)
            s.coll("AllGather", cch, [KIp[g]], [KIg[g]], GROUPS, CC_INC)
            s.coll("AllGather", cch, [KTp[g]], [KTg[g]], GROUPS, CC_INC)
            s.coll("AllGather", cch, [Vp[g]], [Vg[g]], GROUPS, CC_INC)

        build_A(c=c, T=dict(
            after_group=after_group,
            HIN=hin, WIN=WIN[l], G1=G1[l], QT=QT, KT=dummyKT, V=dummyV, QIT=QIT, KIT=dummyKI, WI=WI,
            URT=dummyUR, GUT=GUT, SGR=SGR, SGA=SGA,
            KT_out=lambda g, ck: KTp[g].rearrange("(p h) t -> p h t", h=8)[:, ck, :],
            URT_out=lambda g, ck: URp[g][ck // 4].rearrange("(p c) t -> p c t", c=4)[:, ck % 4, :],
            KIT_out=lambda g: KIp[g][:, :],
            V_out=lambda t: Vp[t // GT][(t % GT) * 128:(t % GT + 1) * 128, :]))
        c.end_phase()

        def ur_in(ck, kc):
            g, o = offs(kc)
            v = URg[g][ck // 4].rearrange("(r p c) t -> r p c t", r=4, c=4)
            return v[:, :, ck % 4, o:o + 128].rearrange("r p k -> p r k")

        def kit_in(kc):
            g, o = offs(kc)
            return KIg[g].rearrange("(r p) t -> r p t", r=4)[:, :, o:o + 128].rearrange("r p k -> p r k")

        def kt_in(r4, kc):
            g, o = offs(kc)
            return KTg[g].rearrange("(r p h) t -> r p h t", r=4, h=8)[r4, :, :, o:o + 128]

        def v_in(kc):
            g, o = offs(kc)
            return Vg[g].rearrange("(r t) f -> r t f", r=4)[:, o:o + 128, :].rearrange("r p f -> p r f")

        build_B(c=c, T=dict(URall=None, CW=CW[l], CBI=CBI[l], WA=WA[l], WX=WX[l], BA=BA[l],
                            BX=BX[l], LAM=LAM[l], HST=HST, UR_in=ur_in))
        c.end_phase()
        CFN(c=c, T=dict(QT=QT, QIT=QIT, WI=WI, KTall=None, Vall=None, KITall=None,
                        CB=CB, TBG=TBG, CH=CH, YAT=YAT, KIT_in=kit_in, KT_in=kt_in, V_in=v_in))
        c.end_phase()
        last = l == nlayer - 1
        build_D(c=c, T=dict(HIN=hin, HSM=HST, RM=RM, GUT=GUT, SGR=SGR, SGA=SGA, YAT=YAT,
                            WOUT=WOUT[l], G2=G2[l], W1=W1[l], W2=W2[l], FG=FG,
                            HOUT=HX if last else H1, FOUT=FOUT if last else FX))
        c.end_phase()
    return c.finish()


def kernel(x, norm1_g, w_in, conv_w, conv_b, w_rg_a, b_rg_a, w_rg_x, b_rg_x, lru_lambda,
                 w_out, norm2_g, w_mlp1, w_mlp2, rel_bias, meta_tokens, final_g):
    f32 = np.float32
    A = lambda a: np.ascontiguousarray(np.asarray(a, f32))
    x = A(x)
    rel_bias = A(rel_bias)
    hs = []
    for b in range(NB):
        h = np.zeros((LP, D), f32)
        h[:NMETA] = A(meta_tokens)
        h[NMETA:L] = x[b]
        hs.extend(to_slots(h))
    fgb = np.ascontiguousarray(np.broadcast_to(A(final_g)[None, :], (128, D)))
    shared = dict(WIN=A(w_in), G1=A(norm1_g), WOUT=A(w_out), G2=A(norm2_g), W1=A(w_mlp1),
                  W2=A(w_mlp2), FG=fgb)
    cols8 = lambda v: np.ascontiguousarray(A(v).reshape(2, 8, 128).transpose(0, 2, 1))
    shared.update(CW=np.ascontiguousarray(A(conv_w).reshape(2, 4, 8, 128).transpose(0, 3, 2, 1)),
                  CBI=cols8(conv_b), WA=A(w_rg_a), WX=A(w_rg_x), BA=cols8(b_rg_a), BX=cols8(b_rg_x),
                  LAM=cols8(lru_lambda))
    in_maps = []
    for cc in range(NCORE):
        b, r = divmod(cc, 4)
        tbg, chh, cb = host_consts_C(rel_bias, r)
        rmask = np.zeros((128, 4), f32)
        rmask[:, r] = 1.0
        m = dict(shared)
        m.update(HIN0=hs[cc], RM=rmask, CB=cb, TBG=tbg, CH=chh)
        in_maps.append(m)
    res = run_spmd(build_fused(), in_maps)
    out = np.zeros((NB, SEQ, D), f32)
    for b in range(NB):
        full = from_slots([res[4 * b + r]["FOUT"] for r in range(4)])
        out[b] = full[NMETA:L]
    return out


SEG = 2048
MASKNEG = 30000.0


def build_C2(nslot=NSLOT, nbis=NBIS, c=None, T=None):
    standalone = c is None
    if standalone:
        c = Ctx()
    s = c.s
    QT = c.io(T, "QT", [128, 8, TOK], BF16, "ExternalInput")
    QIT = c.io(T, "QIT", [128, 4, TOK], BF16, "ExternalInput")
    WI = c.io(T, "WI", [TOK, 8], F32, "ExternalInput")
    KTall = c.io(T, "KTall", [4, 128, 8, TOK], BF16, "ExternalInput")
    Vall = c.io(T, "Vall", [4, TOK, 1032], BF16, "ExternalInput")
    KITall = c.io(T, "KITall", [4, 128, TOK], BF16, "ExternalInput")
    CB = c.io(T, "CB", [128, 512], F32, "ExternalInput")
    TBG = c.io(T, "TBG", [128, 5, 8, 128], F32, "ExternalInput")
    CH = c.io(T, "CH", [128, 8], F32, "ExternalInput")
    YAT = c.io(T, "YAT", [128, 8, TOK], F32, "ExternalOutput")

    idb, b_idb, idf, b_idf = const_identities(c)
    NKMAX = 512 * nslot
    I_sb = [c.sb([128, NKMAX], F32) for _ in range(2)]; b_I = [Buf(), Buf()]
    junk = c.sb([128, SEG], BF16); b_junk = Buf()
    cb_sb = c.sb([128, 512], BF16); b_cb = Buf()
    cbst = None
    ch_sb = c.sb([128, 8], F32); b_ch = Buf()
    tb_sb = c.sb([128, 5, 8, 128], BF16); b_tb = Buf()
    pw2 = c.sb([128, nbis], F32); b_pw2 = Buf()
    negc = c.sb([128, 1], F32); b_negc = Buf()
    q_sb = [c.sb([128, 8, 128], BF16) for _ in range(2)]; b_q = [Buf(), Buf()]
    qi_sb = [c.sb([128, 4, 128], BF16)] * 2; b_qi = [Buf()] * 2
    wi_sb = [c.sb([128, 8], F32)] * 2; b_wi = [Buf()] * 2
    diagw = [c.sb([128, 8, 128], BF16)] * 2; b_dg = [Buf()] * 2
    kit = [c.sb([128, 512], BF16) for _ in range(2)]; b_kit = [Buf(), Buf()]
    NRR = 2
    rr = [c.sb([128, 512], BF16) for _ in range(NRR)]; b_rr = [Buf() for _ in range(NRR)]
    kt = [c.sb([128, 8, 512], BF16) for _ in range(2)]; b_kt = [Buf(), Buf()]
    vv = [c.sb([128, 4, 1032], BF16) for _ in range(2)]; b_vv = [Buf(), Buf()]
    mk = [c.sb([128, 512], BF16)] * 2; b_mk = [Buf()] * 2
    mb = [c.sb([128, 4, 128], BF16) for _ in range(2)]; b_mb = [Buf(), Buf()]
    E = [c.sb([128, 8, 128], BF16) for _ in range(2)]
    b_E = [[Buf(), Buf()] for _ in range(2)]
    y_sb = c.sb([128, 8, 128], F32); b_y = Buf()
    yT = c.sb([128, 8, 128], F32); b_yT = Buf()
    Mx = c.sb([128, 1], F32); b_Mx = Buf()
    Mi = c.sb([128, nbis], F32); b_Mi = Buf()
    cand = c.sb([128, 1], F32); b_cand = Buf()
    cnts = c.sb([128, 16], F32); b_cnts = Buf()
    cnt = c.sb([128, 1], F32); b_cnt = Buf()
    dd = c.sb([128, 1], F32); b_dd = Buf()
    thr = [c.sb([128, 1], F32) for _ in range(2)]; b_thr = [Buf(), Buf()]
    rec = c.sb([128, 8], F32); b_rec = Buf()

    psS = [c.ps([128, 512], F32) for _ in range(2)]; b_psS = [Buf(), Buf()]
    psMT = c.ps([128, 1024], BF16); b_psMT = Buf()
    psR = c.ps([128, 512], F32); b_psR = Buf()
    psI = c.ps([128, 512], F32); b_psI = Buf()
    psO = [c.ps([128, 512], F32) for _ in range(3)]; b_psO = [Buf() for _ in range(3)]

    ch_misc = Chan()
    ch_kit = [Chan(), Chan()]
    ch_kt = [Chan(), Chan()]
    ch_vv = [Chan(), Chan()]
    ch_q = [Chan(), Chan()]
    ch_out = Chan(); b_out = Buf()

    s.dma("sp", ch_misc, y_sb[:, 0:4, :].rearrange("p h q -> p (h q)"), CB[:, :], writes=[b_y])
    s.op("dve", lambda e: e.tensor_copy(out=cb_sb[:], in_=y_sb[:, 0:4, :].rearrange("p h q -> p (h q)")),
         reads=[b_y], writes=[b_cb])
    s.dma("sp", ch_misc, ch_sb[:], CH[:, :], writes=[b_ch])
    s.op("pool", lambda e: e.memset(negc[:], -MASKNEG), writes=[b_negc])
    for j in range(nbis):
        s.op("pool", lambda e, j=j: e.memset(pw2[:, j:j + 1], 2.0 ** (-j)), writes=[b_pw2])
    for rel in range(5):
        s.dma("sp", ch_misc, y_sb[:], TBG[:, rel, :, :], writes=[b_y])
        s.op("dve", lambda e: e.tensor_tensor(
            out=y_sb[:], in0=y_sb[:], in1=ch_sb[:].unsqueeze(2).to_broadcast([128, 8, 128]),
            op=ALU.subtract), reads=[b_y, b_ch], writes=[b_y])
        s.op("dve", lambda e, rel=rel: e.tensor_scalar(
            out=tb_sb[:, rel, :, :], in0=y_sb[:], scalar1=float(128.0 ** 0.5), scalar2=None,
            op0=ALU.mult), reads=[b_y], writes=[b_tb])

    hb = [(0, 3), (3, 6), (6, 8)]
    st = dict(dcount=0, nrr=0, kvc=0, ec=0)

    def gen_indexer(w):
        p = w % 2
        nkc = w + 1
        tsl = slice(w * 128, (w + 1) * 128)
        s.dma("sp", ch_q[p], q_sb[p][:], QT[:, :, tsl], writes=[b_q[p]])
        s.dma("sp", ch_q[p], qi_sb[p][:], QIT[:, :, tsl], writes=[b_qi[p]])
        s.dma("sp", ch_q[p], wi_sb[p][:], WI[tsl, :], writes=[b_wi[p]])
        for h in range(8):
            s.op("dve", lambda e, h=h: e.tensor_scalar(
                out=diagw[p][:, h, :], in0=idb[:], scalar1=wi_sb[p][:, h:h + 1], scalar2=None,
                op0=ALU.mult), reads=[b_wi[p], b_idb], writes=[b_dg[p]])
        for kc in range(nkc):
            ksl = slice(kc * 128, (kc + 1) * 128)
            kb = st["dcount"] % 2
            st["dcount"] += 1
            kin = (T["KIT_in"](kc) if (T is not None and "KIT_in" in T)
                   else KITall[:, :, ksl].rearrange("r p k -> p r k"))
            s.dma("sp", ch_kit[kb], kit[kb][:].rearrange("p (r k) -> p r k", r=4),
                  kin, writes=[b_kit[kb]])
            rbs = []
            for h in range(8):
                po = (h % 2) * 64
                rb = st["nrr"] % NRR
                st["nrr"] += 1
                rbs.append(rb)
                s.op("pe", lambda e, h=h, po=po, kb=kb: e.matmul(
                    psR[:], lhsT=qi_sb[p][po:po + 64, h // 2, :], rhs=kit[kb][po:po + 64, :],
                    start=True, stop=True), reads=[b_qi[p], b_kit[kb]], writes=[b_psR])
                s.op("act", lambda e, rb=rb: e.activation(out=rr[rb][:], in_=psR[:], func=AF.Relu),
                     reads=[b_psR], writes=[b_rr[rb]])
                s.op("pe", lambda e, h=h, rb=rb: e.matmul(
                    psI[:], lhsT=diagw[p][:, h, :], rhs=rr[rb][:], start=(h == 0), stop=(h == 7)),
                    reads=[b_dg[p], b_rr[rb]], writes=[b_psI])
            dst = I_sb[p][:, kc * 512:(kc + 1) * 512]
            s.op("dve", lambda e, dst=dst: e.tensor_copy(out=dst, in_=psI[:]),
                 reads=[b_psI], writes=[b_I[p]])
            yield

    def gen_bisect(w):
        p = w % 2
        nk = 512 * (w + 1)
        Ib = I_sb[p]
        segs = [(a, min(nk, a + SEG)) for a in range(0, nk, SEG)]
        for a, b_ in segs:
            sg = a // SEG
            s.op("dve", lambda e, a=a, b_=b_, sg=sg: e.tensor_reduce(
                out=cnts[:, sg:sg + 1], in_=Ib[:, a:b_], axis=AX.X, op=ALU.max,
                apply_absolute_value=True), reads=[b_I[p]], writes=[b_cnts])
            yield
        s.op("dve", lambda e: e.tensor_reduce(out=Mx[:], in_=cnts[:, 0:len(segs)], axis=AX.X,
                                              op=ALU.max), reads=[b_cnts], writes=[b_Mx])
        last = Ib[:, nk - 512:nk]
        s.op("dve", lambda e: e.tensor_tensor(out=last, in0=last, in1=cb_sb[:], op=ALU.add),
             reads=[b_I[p], b_cb], writes=[b_I[p]])
        s.op("dve", lambda e: e.tensor_scalar(
            out=Mx[:], in0=Mx[:], scalar1=1.001, scalar2=1e-20, op0=ALU.mult, op1=ALU.add),
            reads=[b_Mx], writes=[b_Mx])
        s.op("dve", lambda e: e.tensor_scalar(
            out=Mi[:], in0=pw2[:], scalar1=Mx[:, 0:1], scalar2=None, op0=ALU.mult),
            reads=[b_Mx, b_pw2], writes=[b_Mi])
        s.op("dve", lambda e: e.memset(cand[:], 0.0), writes=[b_cand])
        for it in range(nbis):
            for a, b_ in segs:
                sg = a // SEG
                s.op("dve", lambda e, a=a, b_=b_, sg=sg: e.tensor_scalar(
                    out=junk[:, 0:b_ - a], in0=Ib[:, a:b_], scalar1=cand[:, 0:1], scalar2=None,
                    op0=ALU.is_ge, op1=ALU.add, accum_out=cnts[:, sg:sg + 1]),
                    reads=[b_I[p], b_cand], writes=[b_junk, b_cnts])
                yield
            s.op("dve", lambda e: e.tensor_reduce(out=cnt[:], in_=cnts[:, 0:len(segs)], axis=AX.X,
                                                  op=ALU.add), reads=[b_cnts], writes=[b_cnt])
            lastit = it == nbis - 1
            s.op("dve", lambda e, lastit=lastit: e.tensor_scalar(
                out=dd[:], in0=cnt[:], scalar1=TOPK - 0.5, scalar2=(1.0 if lastit else 0.5),
                op0=ALU.is_ge, op1=ALU.subtract), reads=[b_cnt], writes=[b_dd])
            dst_t, dst_b = (thr[p], b_thr[p]) if lastit else (cand, b_cand)
            s.op("dve", lambda e, it=it, dst_t=dst_t: e.scalar_tensor_tensor(
                out=dst_t[:], in0=dd[:], scalar=Mi[:, it:it + 1], in1=cand[:],
                op0=ALU.mult, op1=ALU.add), reads=[b_dd, b_Mi, b_cand], writes=[dst_b])

    def gen_attention(w):
        p = w % 2
        nkc = w + 1
        nkt = 4 * nkc
        tsl = slice(w * 128, (w + 1) * 128)
        for kc in range(nkc):
            ksl = slice(kc * 128, (kc + 1) * 128)
            kb = st["kvc"] % 2
            st["kvc"] += 1
            for r4 in range(4):
                ktin = (T["KT_in"](r4, kc) if (T is not None and "KT_in" in T)
                        else KTall[r4, :, :, ksl])
                s.dma("sp", ch_kt[kb], kt[kb][:, :, r4 * 128:(r4 + 1) * 128], ktin,
                      writes=[b_kt[kb]])
            vin = (T["V_in"](kc) if (T is not None and "V_in" in T)
                   else Vall[:, ksl, :].rearrange("r p f -> p r f"))
            s.dma("pool", ch_vv[kb], vv[kb][:], vin, writes=[b_vv[kb]])
            s.op("dve", lambda e, kc=kc, kb=kb: e.tensor_scalar(
                out=mk[kb][:], in0=I_sb[p][:, kc * 512:(kc + 1) * 512], scalar1=thr[p][:, 0:1],
                scalar2=None, op0=ALU.is_ge), reads=[b_I[p], b_thr[p]], writes=[b_mk[kb]])
            for r4 in range(4):
                s.op("pe", lambda e, r4=r4, kb=kb: e.transpose(
                    out=psMT[:, r4 * 128:(r4 + 1) * 128], in_=mk[kb][:, r4 * 128:(r4 + 1) * 128],
                    identity=idb[:]), reads=[b_mk[kb], b_idb], writes=[b_psMT])
            s.op("act", lambda e, kb=kb: e.activation(
                out=mb[kb][:].rearrange("p r q -> p (r q)"), in_=psMT[:, 0:512], func=AF.Identity,
                scale=MASKNEG, bias=negc[:, 0:1]), reads=[b_psMT, b_negc], writes=[b_mb[kb]])
            for r4 in range(4):
                ktile = 4 * kc + r4
                rel = 4 * (kc - w) + r4
                eb = st["ec"] % 2
                st["ec"] += 1
                hasb = rel >= -1
                for hg in range(2):
                    for hh in range(4):
                        h = hg * 4 + hh
                        s.op("pe", lambda e, h=h, hg=hg, hh=hh, kb=kb, r4=r4: e.matmul(
                            psS[hg][:, hh * 128:(hh + 1) * 128],
                            lhsT=kt[kb][:, h, r4 * 128:(r4 + 1) * 128], rhs=q_sb[p][:, h, :],
                            start=(hh == 0), stop=False, skip_group_check=True),
                            reads=[b_kt[kb], b_q[p]], writes=[b_psS[hg]])
                    if hasb:
                        s.op("pe", lambda e, hg=hg, rel=rel: e.matmul(
                            psS[hg][:], lhsT=idb[:],
                            rhs=tb_sb[:, rel + 1, hg * 4:(hg + 1) * 4, :].rearrange("p h q -> p (h q)"),
                            start=False, stop=False, skip_group_check=True),
                            reads=[b_tb, b_idb], writes=[b_psS[hg]])
                    s.op("pe", lambda e, hg=hg, kb=kb, r4=r4: e.matmul(
                        psS[hg][:].rearrange("p (h q) -> p h q", h=4), lhsT=idb[:],
                        rhs=mb[kb][:, r4, :].unsqueeze(1).to_broadcast([128, 4, 128]),
                        start=False, stop=True, skip_group_check=True),
                        reads=[b_mb[kb], b_idb], writes=[b_psS[hg]])
                    s.op("act", lambda e, hg=hg, eb=eb: e.activation(
                        out=E[eb][:, hg * 4:(hg + 1) * 4, :].rearrange("p h q -> p (h q)"),
                        in_=psS[hg][:], func=AF.Exp, scale=float(SCALE)),
                        reads=[b_psS[hg]], writes=[b_E[eb][hg]])
                for bk, (h0, h1) in enumerate(hb):
                    for h in range(h0, h1):
                        hh = h - h0
                        s.op("pe", lambda e, h=h, hh=hh, bk=bk, eb=eb, kb=kb, r4=r4,
                             ktile=ktile, h0=h0, h1=h1: e.matmul(
                            psO[bk][:, hh * 129:(hh + 1) * 129],
                            lhsT=E[eb][:, h, :], rhs=vv[kb][:, r4, h * 129:(h + 1) * 129],
                            start=(ktile == 0 and h == h0),
                            stop=(ktile == nkt - 1 and h == h1 - 1), skip_group_check=True),
                            reads=[b_E[eb][h // 4], b_vv[kb]], writes=[b_psO[bk]])
            yield
        for bk, (h0, h1) in enumerate(hb):
            nh = h1 - h0
            ov = psO[bk][:, 0:nh * 129].rearrange("p (h f) -> p h f", f=129)
            s.op("dve", lambda e, ov=ov, h0=h0, h1=h1: e.reciprocal(
                out=rec[:, h0:h1], in_=ov[:, :, 128]), reads=[b_psO[bk]], writes=[b_rec])
            s.op("dve", lambda e, ov=ov, h0=h0, h1=h1, nh=nh: e.tensor_tensor(
                out=y_sb[:, h0:h1, :], in0=ov[:, :, 0:128],
                in1=rec[:, h0:h1].unsqueeze(2).to_broadcast([128, nh, 128]), op=ALU.mult),
                reads=[b_psO[bk], b_rec], writes=[b_y])
        for h in range(8):
            pa = h // 4
            s.op("pe", lambda e, h=h, pa=pa: e.transpose(
                out=psS[pa][:, (h % 4) * 128:(h % 4 + 1) * 128], in_=y_sb[:, h, :],
                identity=idf[:]), reads=[b_y, b_idf], writes=[b_psS[pa]])
        for pa in range(2):
            s.op("act", lambda e, pa=pa: e.copy(
                out=yT[:, pa * 4:(pa + 1) * 4, :].rearrange("p h q -> p (h q)"), in_=psS[pa][:]),
                reads=[b_psS[pa]], writes=[b_yT])
        s.dma("sp", ch_out, YAT[:, :, tsl], yT[:], reads=[b_yT], writes=[b_out])
        yield

    def run_all(g):
        for _ in g:
            pass

    def interleave(ga, na, gb, nb):
        ia = ib = 0
        da = db = False
        while not (da and db):
            if not da and (db or ia * nb <= ib * na):
                try:
                    next(ga)
                    ia += 1
                except StopIteration:
                    da = True
            elif not db:
                try:
                    next(gb)
                    ib += 1
                except StopIteration:
                    db = True

    run_all(gen_indexer(0))
    run_all(gen_bisect(0))
    for w in range(nslot):
        if w + 1 < nslot:
            run_all(gen_indexer(w + 1))
            nseg = -(-512 * (w + 2) // SEG)
            interleave(gen_attention(w), w + 2, gen_bisect(w + 1), nseg * (nbis + 1))
        else:
            run_all(gen_attention(w))
    if not standalone:
        return
    s.wait_all("sp", [b_out])
    return c.finish()


kernel_fused = kernel
```
